# Optimizing a Trainium2 kernel written in Bass

```python
import jax
import jax.numpy as jnp
from jax import lax
import numpy as np

D_MODEL = 1024
BATCH = 4
SEQ = 8192
DEPTH = 2

GRID_W = 64
CTX_LEN = 256
D_CONV = 512
CONV_K = 31
N_NA_HEADS = 8
NA_HEAD_DIM = 64
D_NA = N_NA_HEADS * NA_HEAD_DIM
NA_ROWS = 8
NA_COLS = 16
D_RNN = 512
RNN_BLOCKS = 8
RNN_BLOCK_W = D_RNN // RNN_BLOCKS
RNN_CONV_K = 4
LRU_C = 8.0
N_EXPERTS = 16
N_GROUPS = 4
EXPERTS_PER_GROUP = N_EXPERTS // N_GROUPS
TOP_K = 2
D_EXPERT = 512
N_MOD = 6
IN_SIZES = (D_CONV, D_CONV, D_NA, D_NA, D_NA, D_RNN, D_RNN, D_MODEL, D_MODEL, D_MODEL)
D_IN = sum(IN_SIZES)
EPS = 1e-6
NEG_INF = -1e30

kernel_name = "hybrid_dit_conv_natten_rglru_groupmoe"


def _rmsnorm(x, g):
    xf = x.astype(jnp.float32)
    y = xf * lax.rsqrt(jnp.mean(xf * xf, axis=-1, keepdims=True) + EPS)
    return (y * g.astype(jnp.float32)).astype(x.dtype)


def _layernorm(x, g, b):
    xf = x.astype(jnp.float32)
    mu = jnp.mean(xf, axis=-1, keepdims=True)
    var = jnp.mean(jnp.square(xf - mu), axis=-1, keepdims=True)
    y = (xf - mu) * lax.rsqrt(var + EPS)
    return (y * g.astype(jnp.float32) + b.astype(jnp.float32)).astype(x.dtype)


def _modulate(h, shift, scale):
    return h * (1.0 + scale) + shift


def _split_in(z):
    offs = np.cumsum(IN_SIZES)[:-1].tolist()
    return jnp.split(z, offs, axis=-1)


def _dwconv(x, w, b, pad):
    y = lax.conv_general_dilated(
        x, w[:, None, :].astype(x.dtype), window_strides=(1,), padding=[pad],
        dimension_numbers=("NWC", "WIO", "NWC"), feature_group_count=x.shape[-1])
    return y + b.astype(x.dtype)


def _conformer_conv(a, gate, dw_w, dw_b, ln_g, ln_b, pw_w):
    u = a * jax.nn.sigmoid(gate)
    u = _dwconv(u, dw_w, dw_b, (CONV_K // 2, CONV_K // 2))
    u = jax.nn.silu(_layernorm(u, ln_g, ln_b))
    return u @ pw_w


def _heads(t):
    return t.reshape(t.shape[0], t.shape[1], N_NA_HEADS, NA_HEAD_DIM)


def _context_attention(qc, kc, vc):
    scale = NA_HEAD_DIM ** -0.5
    s = jnp.einsum("bqhd,bkhd->bhqk", qc, kc).astype(jnp.float32) * scale
    p = jax.nn.softmax(s, axis=-1)
    o = jnp.einsum("bhqk,bkhd->bqhd", p, vc.astype(jnp.float32))
    return o.astype(qc.dtype).reshape(qc.shape[0], qc.shape[1], D_NA)


def _neighbourhood_attention(q, k, v, kc, vc, rpb):
    b, s, h, d = q.shape
    rows = s // GRID_W
    kr = min(NA_ROWS, rows)
    scale = d ** -0.5
    qg = q.reshape(b, rows, GRID_W, h, d)
    kg = k.reshape(b, rows, GRID_W, h, d)
    vg = v.reshape(b, rows, GRID_W, h, d)
    col = jnp.arange(GRID_W)
    col_start = jnp.clip(col - NA_COLS // 2, 0, GRID_W - NA_COLS)
    col_mask = (col[None, :] >= col_start[:, None]) & (col[None, :] < col_start[:, None] + NA_COLS)
    dc_idx = jnp.clip(col[None, :] - col[:, None] + NA_COLS - 1, 0, 2 * NA_COLS - 2)
    rpb_f = rpb.astype(jnp.float32)
    vc_f = vc.astype(jnp.float32)
    n_loc = kr * GRID_W

    def one_row(r):
        rs = jnp.clip(r - NA_ROWS // 2, 0, rows - kr)
        qr = lax.dynamic_index_in_dim(qg, r, axis=1, keepdims=False)
        kb = lax.dynamic_slice_in_dim(kg, rs, kr, axis=1)
        vb = lax.dynamic_slice_in_dim(vg, rs, kr, axis=1)
        s_loc = jnp.einsum("bqhd,brkhd->bhqrk", qr, kb).astype(jnp.float32) * scale
        dr = rs + jnp.arange(kr) - r
        bias = rpb_f[:, dr + NA_ROWS - 1][:, :, dc_idx]
        s_loc = s_loc + jnp.transpose(bias, (0, 2, 1, 3))[None]
        s_loc = jnp.where(col_mask[None, None, :, None, :], s_loc, NEG_INF)
        s_ctx = jnp.einsum("bqhd,bchd->bhqc", qr, kc).astype(jnp.float32) * scale
        p = jax.nn.softmax(jnp.concatenate([s_loc.reshape(b, h, GRID_W, n_loc), s_ctx], axis=-1), axis=-1)
        p_loc = p[..., :n_loc].reshape(b, h, GRID_W, kr, GRID_W)
        o = (jnp.einsum("bhqrk,brkhd->bqhd", p_loc, vb.astype(jnp.float32))
             + jnp.einsum("bhqc,bchd->bqhd", p[..., n_loc:], vc_f))
        return o.astype(q.dtype)

    out = lax.map(one_row, jnp.arange(rows))
    return jnp.transpose(out, (1, 0, 2, 3, 4)).reshape(b, s, h * d)


def _block_diag(x, w, bias):
    bsz, length, _ = x.shape
    xb = x.reshape(bsz, length, RNN_BLOCKS, RNN_BLOCK_W)
    return jnp.einsum("blnk,nkj->blnj", xb, w).reshape(bsz, length, D_RNN) + bias


def _rglru_coeffs(u, wa, ba, wx, bx, lam):
    f32 = jnp.float32
    uf = u.astype(f32)
    r = jax.nn.sigmoid(_block_diag(uf, wa.astype(f32), ba.astype(f32)))
    i = jax.nn.sigmoid(_block_diag(uf, wx.astype(f32), bx.astype(f32)))
    log_a = -LRU_C * r * jax.nn.softplus(-lam.astype(f32))
    a = jnp.exp(log_a)
    mult = jnp.sqrt(-jnp.expm1(2.0 * log_a))
    return a, mult * (i * uf)


def _linear_scan(a, bx, h0):
    bx = bx.at[:, 0].add(a[:, 0] * h0)

    def combine(left, right):
        a_l, b_l = left
        a_r, b_r = right
        return a_l * a_r, a_r * b_l + b_r

    _, h = lax.associative_scan(combine, (a, bx), axis=1)
    return h


def _rglru_direction(xc, xl, conv_w, conv_b, wa, ba, wx, bx, lam, with_ctx_out):
    pad = (RNN_CONV_K - 1, 0)
    ac, bc = _rglru_coeffs(_dwconv(xc, conv_w, conv_b, pad), wa, ba, wx, bx, lam)
    al, bl = _rglru_coeffs(_dwconv(xl, conv_w, conv_b, pad), wa, ba, wx, bx, lam)
    hc = _linear_scan(ac, bc, jnp.zeros_like(ac[:, 0]))
    hl = _linear_scan(al, bl, hc[:, -1])
    return hl, (hc if with_ctx_out else None)


def _griffin_merge(gate_branch, h_sum, out_w):
    return (jax.nn.gelu(gate_branch) * h_sum.astype(gate_branch.dtype)) @ out_w


def _moe(h, router_w, router_bias, w1, w3, w2):
    f32 = jnp.float32
    scores = jax.nn.sigmoid(h.astype(f32) @ router_w.astype(f32))
    sel = scores + router_bias.astype(f32)
    grp = sel.reshape(sel.shape[:-1] + (N_GROUPS, EXPERTS_PER_GROUP))
    grp_score = jnp.sum(lax.top_k(grp, TOP_K)[0], axis=-1)
    g_idx = jnp.argmax(grp_score, axis=-1)
    in_grp = jnp.take_along_axis(grp, g_idx[..., None, None], axis=-2)[..., 0, :]
    _, local = lax.top_k(in_grp, TOP_K)
    e_idx = g_idx[..., None] * EXPERTS_PER_GROUP + local
    w_sel = jnp.take_along_axis(scores, e_idx, axis=-1)
    w_sel = w_sel / jnp.sum(w_sel, axis=-1, keepdims=True)
    gate = jnp.einsum("blk,blke->ble", w_sel,
                      jax.nn.one_hot(e_idx, N_EXPERTS, dtype=f32)).astype(h.dtype)
    out = jnp.zeros_like(h)
    for e in range(N_EXPERTS):
        he = jax.nn.silu(h @ w1[e]) * (h @ w3[e])
        out = out + gate[..., e:e + 1] * (he @ w2[e])
    return out


def setup_inputs(seed: int = 0) -> dict:
    key = jax.random.key(seed)
    keys = iter(jax.random.split(key, 48))
    f32 = jnp.float32

    def normal(shape, std=1.0):
        return std * jax.random.normal(next(keys), shape, f32)

    def dense(shape, fan_in, mult=1.0):
        return normal(shape, mult * fan_in ** -0.5)

    def gain(shape):
        return 1.0 + normal(shape, 0.05)

    L = DEPTH
    u = jax.random.uniform(next(keys), (L, 2, D_RNN), f32, 0.9, 0.999)
    a0 = u ** (1.0 / LRU_C)
    return {
        "x": normal((BATCH, SEQ, D_MODEL)),
        "c": normal((BATCH, D_MODEL)),
        "ctx": normal((BATCH, CTX_LEN, D_MODEL)),
        "c_ctx": normal((D_MODEL,)),
        "router_w": dense((D_MODEL, N_EXPERTS), D_MODEL),
        "router_bias": normal((N_EXPERTS,), 0.01),
        "mod_w": dense((L, D_MODEL, N_MOD * D_MODEL), D_MODEL, 0.5),
        "mod_b": normal((L, N_MOD * D_MODEL), 0.02),
        "norm1_g": gain((L, D_MODEL)),
        "norm2_g": gain((L, D_MODEL)),
        "in_w": dense((L, D_MODEL, D_IN), D_MODEL),
        "in_b": normal((L, D_IN), 0.02),
        "conv_dw_w": dense((L, CONV_K, D_CONV), CONV_K),
        "conv_dw_b": normal((L, D_CONV), 0.02),
        "conv_ln_g": gain((L, D_CONV)),
        "conv_ln_b": normal((L, D_CONV), 0.02),
        "conv_pw_w": dense((L, D_CONV, D_MODEL), D_CONV),
        "na_q_g": gain((L, NA_HEAD_DIM)),
        "na_k_g": gain((L, NA_HEAD_DIM)),
        "na_rpb": normal((L, N_NA_HEADS, 2 * NA_ROWS - 1, 2 * NA_COLS - 1), 0.1),
        "na_out_w": dense((L, D_NA, D_MODEL), D_NA),
        "rnn_conv_w": dense((L, 2, RNN_CONV_K, D_RNN), RNN_CONV_K),
        "rnn_conv_b": normal((L, 2, D_RNN), 0.02),
        "rnn_wa": dense((L, 2, RNN_BLOCKS, RNN_BLOCK_W, RNN_BLOCK_W), RNN_BLOCK_W),
        "rnn_ba": normal((L, 2, D_RNN), 0.02),
        "rnn_wx": dense((L, 2, RNN_BLOCKS, RNN_BLOCK_W, RNN_BLOCK_W), RNN_BLOCK_W),
        "rnn_bx": normal((L, 2, D_RNN), 0.02),
        "rnn_lam": jnp.log(a0) - jnp.log1p(-a0),
        "rnn_out_w": dense((L, D_RNN, D_MODEL), D_RNN),
        "out_w": dense((L, D_MODEL, D_MODEL), D_MODEL),
        "exp_w1": dense((L, N_EXPERTS, D_MODEL, D_EXPERT), D_MODEL),
        "exp_w3": dense((L, N_EXPERTS, D_MODEL, D_EXPERT), D_MODEL),
        "exp_w2": dense((L, N_EXPERTS, D_EXPERT, D_MODEL), D_EXPERT),
    }


def reference(x, c, ctx, c_ctx, router_w, router_bias, mod_w, mod_b, norm1_g, norm2_g,
              in_w, in_b, conv_dw_w, conv_dw_b, conv_ln_g, conv_ln_b, conv_pw_w,
              na_q_g, na_k_g, na_rpb, na_out_w, rnn_conv_w, rnn_conv_b, rnn_wa, rnn_ba,
              rnn_wx, rnn_bx, rnn_lam, rnn_out_w, out_w, exp_w1, exp_w3, exp_w2):
    c_act = jax.nn.silu(c)
    cctx_act = jax.nn.silu(c_ctx)
    xl, xc = x, ctx
    for l in range(DEPTH):
        ctx_out = l < DEPTH - 1
        mod_l = (c_act @ mod_w[l] + mod_b[l])[:, None, :]
        mod_c = cctx_act @ mod_w[l] + mod_b[l]
        sh1_l, sc1_l, g1_l, sh2_l, sc2_l, g2_l = jnp.split(mod_l, N_MOD, axis=-1)
        sh1_c, sc1_c, g1_c, sh2_c, sc2_c, g2_c = jnp.split(mod_c, N_MOD, axis=-1)

        hl = _modulate(_rmsnorm(xl, norm1_g[l]), sh1_l, sc1_l)
        hc = _modulate(_rmsnorm(xc, norm1_g[l]), sh1_c, sc1_c)
        a_l, ga_l, q_l, k_l, v_l, rx_l, rg_l, gcv_l, gna_l, grn_l = _split_in(hl @ in_w[l] + in_b[l])
        a_c, ga_c, q_c, k_c, v_c, rx_c, rg_c, gcv_c, gna_c, grn_c = _split_in(hc @ in_w[l] + in_b[l])

        conv_l = _conformer_conv(a_l, ga_l, conv_dw_w[l], conv_dw_b[l], conv_ln_g[l],
                                 conv_ln_b[l], conv_pw_w[l])

        kn_c = _rmsnorm(_heads(k_c), na_k_g[l])
        vh_c = _heads(v_c)
        na_l = _neighbourhood_attention(_rmsnorm(_heads(q_l), na_q_g[l]),
                                        _rmsnorm(_heads(k_l), na_k_g[l]), _heads(v_l),
                                        kn_c, vh_c, na_rpb[l]) @ na_out_w[l]

        fwd = (rnn_conv_w[l, 0], rnn_conv_b[l, 0], rnn_wa[l, 0], rnn_ba[l, 0],
               rnn_wx[l, 0], rnn_bx[l, 0], rnn_lam[l, 0])
        bwd = (rnn_conv_w[l, 1], rnn_conv_b[l, 1], rnn_wa[l, 1], rnn_ba[l, 1],
               rnn_wx[l, 1], rnn_bx[l, 1], rnn_lam[l, 1])
        hf_l, hf_c = _rglru_direction(rx_c, rx_l, *fwd, ctx_out)
        hb_l, hb_c = _rglru_direction(rx_c[:, ::-1], rx_l[:, ::-1], *bwd, ctx_out)
        rnn_l = _griffin_merge(rg_l, hf_l + hb_l[:, ::-1], rnn_out_w[l])

        y_l = (jax.nn.sigmoid(gcv_l) * conv_l + jax.nn.sigmoid(gna_l) * na_l
               + jax.nn.sigmoid(grn_l) * rnn_l) @ out_w[l]
        xl = xl + g1_l * y_l
        if ctx_out:
            conv_c = _conformer_conv(a_c, ga_c, conv_dw_w[l], conv_dw_b[l], conv_ln_g[l],
                                     conv_ln_b[l], conv_pw_w[l])
            na_c = _context_attention(_rmsnorm(_heads(q_c), na_q_g[l]), kn_c, vh_c) @ na_out_w[l]
            rnn_c = _griffin_merge(rg_c, hf_c + hb_c[:, ::-1], rnn_out_w[l])
            y_c = (jax.nn.sigmoid(gcv_c) * conv_c + jax.nn.sigmoid(gna_c) * na_c
                   + jax.nn.sigmoid(grn_c) * rnn_c) @ out_w[l]
            xc = xc + g1_c * y_c

        hl2 = _modulate(_rmsnorm(xl, norm2_g[l]), sh2_l, sc2_l)
        xl = xl + g2_l * _moe(hl2, router_w, router_bias, exp_w1[l], exp_w3[l], exp_w2[l])
        if ctx_out:
            hc2 = _modulate(_rmsnorm(xc, norm2_g[l]), sh2_c, sc2_c)
            xc = xc + g2_c * _moe(hc2, router_w, router_bias, exp_w1[l], exp_w3[l], exp_w2[l])
    return xl
```

```python
import contextlib
import numpy as np
import concourse.bass as bass
import concourse.mybir as mybir
from concourse.bass_utils import run_bass_kernel_spmd

F32 = mybir.dt.float32
BF16 = mybir.dt.bfloat16
AF = mybir.ActivationFunctionType
ALU = mybir.AluOpType
AX = mybir.AxisListType

D = 1024; SEQ = 8192; CTX = 256; TT = SEQ + CTX; DEPTH = 2
DIN = 6656; NE = 16; DE = 512
EPS = 1e-6
NEG = -30000.0
O_A, O_GA, O_Q, O_K, O_V, O_RX, O_RG, O_GCV, O_GNA, O_GRN = 0, 512, 1024, 1536, 2048, 2560, 3072, 3584, 4608, 5632

SP_FIELDS = [("mod_b", 48), ("n1g", 8), ("n2g", 8), ("in_b", 52), ("cdw", 124), ("cdb", 4), ("clg", 4), ("clb", 4),
             ("qg", 1), ("kg", 1), ("rcw", 32), ("rcb", 8), ("rba", 8), ("rbx", 8), ("lam", 8)]
SP_OFF = {}
_o = 0
for _n, _w in SP_FIELDS:
    SP_OFF[_n] = (_o, _w); _o += _w
SP_W = _o


def _fm(v):
    return np.ascontiguousarray(v.reshape(-1, 128).T)


def pack_small(inp, l, rev=False):
    sp = np.zeros((128, SP_W), np.float32)
    dsl = slice(None, None, -1) if rev else slice(None)
    def put(name, arr):
        o, w = SP_OFF[name]; assert arr.shape == (128, w), (name, arr.shape); sp[:, o:o + w] = arr
    put("mod_b", _fm(inp["mod_b"][l])); put("n1g", _fm(inp["norm1_g"][l])); put("n2g", _fm(inp["norm2_g"][l]))
    put("in_b", _fm(inp["in_b"][l]))
    cw = inp["conv_dw_w"][l][dsl]
    put("cdw", np.ascontiguousarray(cw.T.reshape(4, 128, 31).transpose(1, 0, 2)).reshape(128, 124))
    put("cdb", _fm(inp["conv_dw_b"][l])); put("clg", _fm(inp["conv_ln_g"][l])); put("clb", _fm(inp["conv_ln_b"][l]))
    put("qg", np.tile(inp["na_q_g"][l], 2)[:, None]); put("kg", np.tile(inp["na_k_g"][l], 2)[:, None])
    rw = inp["rnn_conv_w"][l][dsl]
    put("rcw", np.ascontiguousarray(rw.transpose(2, 0, 1).reshape(4, 128, 2, 4).transpose(1, 2, 0, 3)).reshape(128, 32))
    def dg(a):
        return np.ascontiguousarray(a.reshape(2, 4, 128).transpose(2, 0, 1)).reshape(128, 8)
    put("rcb", dg(inp["rnn_conv_b"][l][dsl])); put("rba", dg(inp["rnn_ba"][l][dsl])); put("rbx", dg(inp["rnn_bx"][l][dsl]))
    put("lam", dg(inp["rnn_lam"][l][dsl]))
    return sp


def na_geometry():
    types = {}; plan = []
    for p in range(64):
        rs0 = min(max(2 * p - 4, 0), 120); rs1 = min(max(2 * p + 1 - 4, 0), 120)
        lo = min(rs0, rs1); hi = max(rs0, rs1) + 7
        lst = []
        for c in range(lo // 2, hi // 2 + 1):
            key = (c - p, rs0 - 2 * p, rs1 - (2 * p + 1))
            if key not in types:
                types[key] = len(types)
            lst.append((c, types[key]))
        plan.append(lst)
    return types, plan


def na_true_tile(rpb, p, c):
    k = np.arange(128)[:, None]; q = np.arange(128)[None, :]
    krow = 2 * c + k // 64; kc = k % 64; qrow = 2 * p + q // 64; qc = q % 64
    rs = np.clip(qrow - 4, 0, 120); cs = np.clip(qc - 8, 0, 48)
    valid = (krow >= rs) & (krow < rs + 8) & (kc >= cs) & (kc < cs + 16)
    dr = np.clip(krow - qrow + 7, 0, 14); dc = np.clip(kc - qc + 15, 0, 30)
    val = rpb[:, dr, dc]
    return np.where(valid[None], val, np.float32(NEG)).astype(np.float32)


def build_na_bias(rpb, rev=False):
    types, plan = na_geometry()
    H = rpb.shape[0]
    out = np.full((H, len(types), 128, 128), NEG, np.float32)
    done = set()
    for p in range(64):
        for (c, tid) in plan[p]:
            if tid in done: continue
            done.add(tid)
            out[:, tid] = na_true_tile(rpb, 63 - p, 63 - c)[:, ::-1, ::-1] if rev else na_true_tile(rpb, p, c)
    return out


class Res:
    __slots__ = ("name", "w", "r")
    def __init__(self, name):
        self.name = name; self.w = None; self.r = {}


class Tl:
    __slots__ = ("t", "res")
    def __init__(self, t, res):
        self.t = t; self.res = res
    def __getitem__(self, idx):
        return self.t[idx]


class Sched:
    SEM_LIMIT = 30000
    def __init__(self, nc, stack, n_sems=84, dma_pool=10):
        self.nc = nc
        self.engs = {"pe": nc.tensor, "dve": nc.vector, "act": nc.scalar, "pool": nc.gpsimd, "sp": nc.sync}
        self.free_sems = [stack.enter_context(nc.semaphore(f"s{i}")) for i in range(n_sems)]
        self.q = {k: [] for k in self.engs}
        self.sem = {k: self.free_sems.pop() for k in self.engs}
        self.cnt = {k: 0 for k in self.engs}
        self.waited = {k: {} for k in self.engs}
        self.pend = {k: ([], []) for k in self.engs}
        self.dma_sems = {k: [[self.free_sems.pop(), 0] for _ in range(dma_pool)] for k in ("sp", "pool", "act")}
        self.dma_rr = {k: 0 for k in self.dma_sems}
        self.last_ev = {}
        self.nres = 0

    def res(self, name=None):
        self.nres += 1
        return Res(name or f"r{self.nres}")

    def _wait(self, eng, ev):
        sem, val = ev
        key = id(sem)
        if self.waited[eng].get(key, 0) >= val:
            return
        self.waited[eng][key] = val
        self.q[eng].append(lambda e, sem=sem, val=val: e.wait_ge(sem, val))

    def _deps(self, eng, reads, writes, skip_same):
        deps = {}
        def add(ev):
            if ev is None: return
            k = id(ev[0])
            if k not in deps or deps[k][1] < ev[1]: deps[k] = ev
        for r in reads: add(r.w)
        for w in writes:
            add(w.w)
            for ev in w.r.values(): add(ev)
        own = id(self.sem[eng])
        for k, ev in deps.items():
            if skip_same and k == own: continue
            self._wait(eng, ev)

    def _commit(self, ev, reads, writes):
        for w in writes:
            w.w = ev; w.r = {}
        for r in reads:
            k = id(ev[0])
            if k not in r.r or r.r[k][1] < ev[1]: r.r[k] = ev

    def op(self, eng, meth, kw, reads=(), writes=(), sig=True):
        self._deps(eng, reads, writes, skip_same=(eng == "pe"))
        fn = lambda e, meth=meth, kw=kw: getattr(e, meth)(**kw)
        if not sig:
            self.pend[eng][0].extend(reads); self.pend[eng][1].extend(writes)
            self.q[eng].append(lambda e, fn=fn: fn(e))
            return None
        if self.cnt[eng] >= self.SEM_LIMIT:
            self.sem[eng] = self.free_sems.pop(); self.cnt[eng] = 0
        self.cnt[eng] += 1
        sem = self.sem[eng]; ev = (sem, self.cnt[eng])
        self.q[eng].append(lambda e, fn=fn, sem=sem: fn(e).then_inc(sem, 1))
        pr, pw = self.pend[eng]
        self._commit(ev, list(reads) + pr, list(writes) + pw)
        self.pend[eng] = ([], [])
        self.last_ev[eng] = ev
        return ev

    def dma(self, qn, out, in_, reads=(), writes=()):
        self._deps(qn, reads, writes, skip_same=False)
        slot = self.dma_sems[qn][self.dma_rr[qn] % len(self.dma_sems[qn])]
        self.dma_rr[qn] += 1
        if slot[1] >= self.SEM_LIMIT:
            slot[0] = self.free_sems.pop(); slot[1] = 0
        if slot[1] > 0:
            self._wait(qn, (slot[0], slot[1]))
        slot[1] += 16
        sem = slot[0]; ev = (sem, slot[1])
        self.q[qn].append(lambda e, out=out, in_=in_, sem=sem: e.dma_start(out=out, in_=in_).then_inc(sem, 16))
        self._commit(ev, reads, writes)
        return ev

    def barrier(self):
        evs = [ev for ev in self.last_ev.values()]
        for qn, slots in self.dma_sems.items():
            for s in slots:
                if s[1] > 0: evs.append((s[0], s[1]))
        for eng in self.engs:
            for ev in evs:
                if id(ev[0]) == id(self.sem[eng]) and eng == "pe": continue
                self._wait(eng, ev)

    def emit(self):
        with self.nc.Block() as block:
            for name, attr in (("pe", "tensor"), ("dve", "vector"), ("act", "scalar"), ("pool", "gpsimd"), ("sp", "sync")):
                q = self.q[name]
                def body(e, q=q):
                    for f in q: f(e)
                getattr(block, attr)(body)


SUBT = [(0, CTX, 1)] + [(CTX + 512 * i, 512, 0) for i in range(16)]
HALF = SEQ // 2; NOWN = HALF // 512
VW = 8 * 65


def build(upto="all", dump=()):
    nc = bass.Bass("TRN2", target_bir_lowering=False)
    def din(name, shape, dt=F32):
        return nc.dram_tensor(name, list(shape), dt, kind="ExternalInput").ap()
    def dscr(name, shape, dt):
        return nc.dram_tensor(name, list(shape), dt, kind="Internal").ap()
    xT = din("xT", [D, TT]); c_pk = din("c_pk", [128, 16]); ident_d = din("ident_in", [128, 128])
    sp_d = din("sp", [DEPTH, 128, SP_W]); inbv_d = din("inbv", [DEPTH, 128, 512]); rbias_d = din("rbias", [128, 4 * NE])
    mod_w = din("mod_w", [DEPTH, D, 6 * D]); in_w = din("in_w", [DEPTH, D, DIN])
    conv_pw = din("conv_pw_w", [DEPTH, 512, D]); na_out = din("na_out_w", [DEPTH, 512, D])
    rnn_out = din("rnn_out_w", [DEPTH, 512, D]); out_w = din("out_w", [DEPTH, D, D])
    w1_d = din("exp_w1", [DEPTH, NE, D, DE]); w3_d = din("exp_w3", [DEPTH, NE, D, DE]); w2_d = din("exp_w2", [DEPTH, NE, DE, D])
    router_w = din("router_w", [D, NE]); rnnblk_d = din("rnnblk", [DEPTH, 2, 2, 4, 128, 128])
    natypes, naplan = na_geometry(); ntypes = len(natypes)
    nab_d = din("nabias", [DEPTH, 8, ntypes, 128, 128]); sel_d = din("sel", [NE, NE * 128])
    outT = nc.dram_tensor("outT", [D, HALF], F32, kind="ExternalOutput").ap()
    U = dscr("U", [512, TT], BF16); QN = dscr("QN", [512, TT], BF16); KN = dscr("KN", [512, TT], BF16)
    V = dscr("V", [TT, VW], BF16); RX = dscr("RX", [512, TT], BF16); RG = dscr("RG", [512, TT], BF16)
    G3 = dscr("G3", [3 * D, TT], BF16); UC = dscr("UC", [512, TT], BF16); NA = dscr("NA", [512, TT], BF16)
    RN = dscr("RN", [512, TT], BF16); XM = dscr("XM", [D, TT], F32); XN = dscr("XN", [D, TT], F32)
    H2 = dscr("H2", [D, TT], BF16); GT = dscr("GT", [NE, TT], F32)
    scr = dict(H2=H2, GT=GT, U=U, QN=QN, KN=KN, V=V, RX=RX, RG=RG, G3=G3, UC=UC, NA=NA, RN=RN, XM=XM, XN=XN)
    dump_out = {n: nc.dram_tensor("dump_" + n, list(scr[n].shape), scr[n].dtype, kind="ExternalOutput").ap() for n in dump}

    with contextlib.ExitStack() as top:
        S = Sched(nc, top)
        uid = [0]
        def sb(stack, name, shape, dt):
            uid[0] += 1
            return Tl(stack.enter_context(nc.sbuf_tensor(f"{name}_{uid[0]}", list(shape), dt)), S.res(name))
        def pst(stack, name, shape=(128, 512), dt=F32):
            uid[0] += 1
            return Tl(stack.enter_context(nc.psum_tensor(f"{name}_{uid[0]}", list(shape), dt)), S.res(name))
        def R(ts): return [t.res if hasattr(t, 'res') else t for t in ts]
        def ACT(out, in_, func, rd, wr, **kw):
            S.op("act", "activation", dict(out=out, in_=in_, func=func, **kw), R(rd), R(wr))
        def MM(out, lhsT, rhs, start, stop, rd, wr):
            S.op("pe", "matmul", dict(out=out, lhsT=lhsT, rhs=rhs, start=start, stop=stop), R(rd), R(wr), sig=stop)
        def TT_(eng, out, in0, in1, op, rd, wr):
            S.op(eng, "tensor_tensor", dict(out=out, in0=in0, in1=in1, op=op), R(rd), R(wr))
        def STT(out, in0, scalar, in1, op0, op1, rd, wr):
            S.op("dve", "scalar_tensor_tensor", dict(out=out, in0=in0, scalar=scalar, in1=in1, op0=op0, op1=op1), R(rd), R(wr))
        def TS(eng, out, in0, s1, s2, op0, op1, rd, wr):
            kw = dict(out=out, in0=in0, scalar1=s1, scalar2=s2, op0=op0)
            if op1 is not None: kw["op1"] = op1
            S.op(eng, "tensor_scalar", kw, R(rd), R(wr))
        def CP(eng, out, in_, rd, wr):
            S.op(eng, "tensor_copy", dict(out=out, in_=in_), R(rd), R(wr))
        def MS(eng, ap, val, wr):
            S.op(eng, "memset", dict(ap=ap, constant=val), [], R(wr))
        def DMA(q, out, in_, rd=(), wr=()):
            S.dma(q, out, in_, R(rd), R(wr))
        class RR:
            def __init__(self, lst): self.l = lst; self.i = -1
            def __call__(self):
                self.i += 1; return self.l[self.i % len(self.l)]

        ident = sb(top, "ident", [128, 128], F32); identb = sb(top, "identb", [128, 128], BF16)
        onesb = sb(top, "onesb", [128, 128], BF16)
        blk64 = sb(top, "blk64", [128, 128], BF16)
        spt = sb(top, "spt", [128, DEPTH, SP_W], F32)
        cact = sb(top, "cact", [128, 8, 2], F32)
        modv = sb(top, "modv", [128, DEPTH, 2, 48], F32)
        gs1 = sb(top, "gs1", [128, DEPTH, 2, 8], F32); gs2 = sb(top, "gs2", [128, DEPTH, 2, 8], F32)
        c8 = sb(top, "c8", [128, DEPTH, 8], F32); c16 = sb(top, "c16", [128, DEPTH, 8], F32)
        def spc(l, name, i=0, w=1):
            o, _ = SP_OFF[name]; return spt[:, l, o + i:o + i + w]

        def load_W(l, stack):
            W = sb(stack, "W", [128, 8, DIN], BF16)
            Ws = [S.res(f"W{s}") for s in range(13)]
            order = list(range(13)) if l < DEPTH - 1 else [5, 3, 4, 0, 1, 2, 6, 7, 8, 9, 10, 11, 12]
            for s in order:
                DMA("pool", W[:, :, s * 512:(s + 1) * 512], in_w[l].rearrange("(k p) n -> p k n", p=128)[:, :, s * 512:(s + 1) * 512], wr=[Ws[s]])
            return W, Ws

        w0stack = top.enter_context(contextlib.ExitStack())
        preW0 = load_W(0, w0stack)
        with contextlib.ExitStack() as ph:
            ps0 = pst(ph, "ps0")
            mwb = RR([sb(ph, f"mwb{i}", [128, 8, 512], F32) for i in range(2)])
            ctmp = sb(ph, "ctmp", [128, 16], F32)
            DMA("sp", ident[:], ident_d[:, :], wr=[ident])
            DMA("sp", spt[:], sp_d.rearrange("l p w -> p l w"), wr=[spt])
            DMA("sp", ctmp[:], c_pk[:, :], wr=[ctmp])
            CP("dve", identb[:], ident[:], [ident], [identb])
            MS("dve", onesb[:], 1.0, [onesb]); MS("dve", blk64[:], 0.0, [blk64])
            MS("dve", blk64[0:64, 0:64], 1.0, [blk64]); MS("dve", blk64[64:128, 64:128], 1.0, [blk64])
            ACT(cact[:].rearrange("p k j -> p (k j)"), ctmp[:], AF.Silu, [ctmp], [cact])
            for l in range(DEPTH):
                ACT(c8[:, l, :], spc(l, "lam", 0, 8), AF.Exp, [spt], [c8], scale=-1.0)
                ACT(c8[:, l, :], c8[:, l, :], AF.Ln, [c8], [c8], bias=1.0, scale=1.0)
                TS("dve", c16[:, l, :], c8[:, l, :], -16.0, None, ALU.mult, None, [c8], [c16])
                TS("dve", c8[:, l, :], c8[:, l, :], -8.0, None, ALU.mult, None, [c8, c16], [c8])
            for l in range(DEPTH):
                for s in range(12):
                    buf = mwb()
                    DMA("sp", buf[:], mod_w[l].rearrange("(k p) n -> p k n", p=128)[:, :, s * 512:(s + 1) * 512], wr=[buf])
                    for g in range(4):
                        gg = s * 4 + g
                        for k in range(8):
                            MM(ps0[:, 2 * gg:2 * gg + 2], buf[:, k, g * 128:(g + 1) * 128], cact[:, k, :], k == 0, k == 7, [buf, cact], [ps0])
                for j in range(2):
                    TT_("dve", modv[:, l, j, :], ps0[:, j:96:2], spc(l, "mod_b", 0, 48), ALU.add, [ps0, spt], [modv])
                    STT(gs1[:, l, j, :], modv[:, l, j, 8:16], 1.0, spc(l, "n1g", 0, 8), ALU.add, ALU.mult, [modv, spt], [gs1])
                    STT(gs2[:, l, j, :], modv[:, l, j, 32:40], 1.0, spc(l, "n2g", 0, 8), ALU.add, ALU.mult, [modv, spt], [gs2])
            S.barrier()

        def rstd_from_ss(ss_ps, n, rs, inv_n):
            ACT(rs[:, 0:n], ss_ps[:, 0:n], AF.Ln, [ss_ps], [rs], scale=inv_n, bias=EPS)
            ACT(rs[:, 0:n], rs[:, 0:n], AF.Exp, [rs], [rs], scale=-0.5)

        def phase1(l, Xsrc, pre=None):
            with contextlib.ExitStack() as ph:
                W, Ws = pre if pre is not None else load_W(l, ph)
                inbv = sb(ph, "inbv", [128, 512], F32)
                DMA("sp", inbv[:], inbv_d[l], wr=[inbv])
                X = sb(ph, "X", [128, 8, 512], F32); sq = sb(ph, "sq", [128, 8, 512], BF16)
                rs = sb(ph, "rs", [128, 512], F32); hh = [sb(ph, f"h{i}", [128, 8, 512], BF16) for i in range(2)]
                hres = {id(hh[b_]): [S.res(f"h{b_}_{k}") for k in range(8)] for b_ in range(2)}
                pss = pst(ph, "pss"); pq = pst(ph, "pq")
                pz = RR([pst(ph, f"pz{i}") for i in range(5)])
                stg = RR([sb(ph, f"stg{i}", [128, 512], BF16) for i in range(6)])
                vst = RR([sb(ph, f"vst{i}", [128, 8, 65], BF16) for i in range(2)])
                for t in vst.l: MS("dve", t[:], 1.0, [t])
                qset = RR([(sb(ph, f"qb{i}", [128, 512], F32), sb(ph, f"qs{i}", [128, 512], BF16), sb(ph, f"qr{i}", [128, 512], F32)) for i in range(4)])
                sg = RR([sb(ph, f"sg{i}", [128, 512], F32) for i in range(2)])
                def inb(g): return spc(l, "in_b", g, 1)
                Xs = [S.res(f"X{k}") for k in range(8)]
                def loadX(i):
                    t0, n, j = SUBT[i]
                    S.dma("sp", X[:, :, 0:n], Xsrc[:, t0:t0 + n].rearrange("(k p) t -> p k t", p=128), [], Xs)
                def square(i):
                    t0, n, j = SUBT[i]
                    S.op("act", "activation", dict(out=sq[:, :, 0:n], in_=X[:, :, 0:n], func=AF.Square), Xs, [sq.res])
                def prologue(i, h):
                    t0, n, j = SUBT[i]
                    for k in range(8):
                        MM(pss[:, 0:n], onesb[:], sq[:, k, 0:n], k == 0, k == 7, [onesb, sq], [pss])
                    rstd_from_ss(pss, n, rs, 1.0 / D)
                    for k in range(8):
                        S.op("dve", "tensor_tensor", dict(out=X[:, k, 0:n], in0=X[:, k, 0:n], in1=rs[:, 0:n], op=ALU.mult), [Xs[k], rs.res], [Xs[k]])
                        S.op("act", "activation", dict(out=h[:, k, 0:n], in_=X[:, k, 0:n], func=AF.Identity, scale=gs1[:, l, j, k:k + 1], bias=modv[:, l, j, k:k + 1]),
                             [Xs[k], gs1.res, modv.res], [hres[id(h)][k]])
                NT = len(SUBT)
                ALLW = ("U", "Q", "K", "V", "RX", "RG", "G3")
                def wants(i):
                    if l < DEPTH - 1: return ALLW
                    if i == 0: return ("K", "V", "RX")
                    if i <= NOWN: return ALLW
                    if i == NOWN + 1: return ("U", "K", "V", "RX")
                    return ("RX",)
                def main(i, h, sq_next):
                    t0, n, j = SUBT[i]; want = wants(i)
                    def zmm(ps, col0, n):
                        for k in range(8):
                            MM(ps[:, 0:n], W[:, k, col0:col0 + 128], h[:, k, 0:n], k == 0, k == 7, [Ws[col0 // 512], hres[id(h)][k]], [ps])
                    for g in range(4 if "U" in want else 0):
                        pa = pz(); zmm(pa, O_A + g * 128, n)
                        pg = pz(); zmm(pg, O_GA + g * 128, n)
                        s_ = sg(); st = stg()
                        ACT(s_[:, 0:n], pg[:, 0:n], AF.Sigmoid, [pg, spt], [s_], bias=inb(4 + g), scale=1.0)
                        STT(st[:, 0:n], pa[:, 0:n], inb(g), s_[:, 0:n], ALU.add, ALU.mult, [pa, s_, spt], [st])
                        DMA("sp", U[g * 128:(g + 1) * 128, t0:t0 + n], st[:, 0:n], rd=[st])
                    pend = []
                    def finish(b_, s2, r_, st, gname, extra, dst, g):
                        MM(pq[:, 0:n], blk64[:], s2[:, 0:n], True, True, [blk64, s2], [pq])
                        rstd_from_ss(pq, n, r_, 1.0 / 64)
                        STT(b_[:, 0:n], b_[:, 0:n], spc(l, gname, 0, 1), r_[:, 0:n], ALU.mult, ALU.mult, [b_, r_, spt], [b_])
                        TS("pool", st[:, 0:n], b_[:, 0:n], extra, 1.0, ALU.mult, ALU.mult, [b_], [st])
                        DMA("sp", dst[g * 128:(g + 1) * 128, t0:t0 + n], st[:, 0:n], rd=[st])
                    for col0, gname, dst, extra in ((O_Q, "qg", QN, 0.125), (O_K, "kg", KN, 1.0)):
                        if ("Q" if col0 == O_Q else "K") not in want: continue
                        for g in range(4):
                            pa = pz(); zmm(pa, col0 + g * 128, n)
                            b_, s2, r_ = qset(); st = stg()
                            bias_ap = inb(col0 // 128 + g)
                            ACT(b_[:, 0:n], pa[:, 0:n], AF.Identity, [pa, spt], [b_], bias=bias_ap, scale=1.0)
                            ACT(s2[:, 0:n], pa[:, 0:n], AF.Square, [pa, spt], [s2], bias=bias_ap, scale=1.0)
                            pend.append((b_, s2, r_, st, gname, extra, dst, g))
                            if len(pend) > 2: finish(*pend.pop(0))
                    for tb in range(n // 128 if "V" in want else 0):
                        pa = pz()
                        for k in range(8):
                            MM(pa[:, :], h[:, k, tb * 128:(tb + 1) * 128], W[:, k, O_V:O_V + 512], k == 0, k == 7, [Ws[O_V // 512], hres[id(h)][k]], [pa])
                        if pend: finish(*pend.pop(0))
                        vt = vst()
                        TT_("dve", vt[:, :, 0:64], pa[:].rearrange("p (h d) -> p h d", h=8), inbv[:].rearrange("p (h d) -> p h d", h=8), ALU.add, [pa, inbv], [vt])
                        DMA("sp", V[t0 + tb * 128:t0 + (tb + 1) * 128, :], vt[:].rearrange("p h d -> p (h d)"), rd=[vt])
                    while pend: finish(*pend.pop(0))
                    if sq_next is not None: square(sq_next)
                    for wn_, col0, func, dst, ng in (("RX", O_RX, AF.Identity, RX, 4), ("RG", O_RG, AF.Gelu_apprx_tanh, RG, 4), ("G3", O_GCV, AF.Sigmoid, G3, 24)):
                        if wn_ not in want: continue
                        for g in range(ng):
                            pa = pz(); zmm(pa, col0 + g * 128, n)
                            st = stg()
                            ACT(st[:, 0:n], pa[:, 0:n], func, [pa, spt], [st], bias=inb(col0 // 128 + g), scale=1.0)
                            DMA("sp", dst[g * 128:(g + 1) * 128, t0:t0 + n], st[:, 0:n], rd=[st])
                seq_ = list(range(NT)) if l < DEPTH - 1 else [0] + list(range(NOWN + 2, NT)) + [NOWN + 1] + list(range(1, NOWN + 1))
                loadX(seq_[0]); square(seq_[0]); prologue(seq_[0], hh[0]); loadX(seq_[1]); square(seq_[1])
                for q_ in range(NT):
                    if q_ + 1 < NT:
                        prologue(seq_[q_ + 1], hh[(q_ + 1) % 2])
                        if q_ + 2 < NT: loadX(seq_[q_ + 2])
                    main(seq_[q_], hh[q_ % 2], seq_[q_ + 2] if q_ + 2 < NT else None)
                S.barrier()

        def phase2_conv(l):
            with contextlib.ExitStack() as ph:
                dg = sb(ph, "dg", [128, 4, 31, 128], BF16)
                for g in range(4):
                    for k in range(31):
                        TS("dve", dg[:, g, k, :], identb[:], spc(l, "cdw", g * 31 + k, 1), None, ALU.mult, None, [identb, spt], [dg])
                upad = sb(ph, "upad", [128, 4, SEQ + 30], BF16)
                py = [pst(ph, f"py{g}") for g in range(4)]
                pm = pst(ph, "pm"); pv = pst(ph, "pv")
                yf = sb(ph, "yf", [128, 4, 512], F32); yb = sb(ph, "yb", [128, 4, 512], BF16); ysq = sb(ph, "ysq", [128, 4, 512], BF16)
                mean = sb(ph, "mean", [128, 512], F32); var = sb(ph, "var", [128, 512], F32); m2 = sb(ph, "m2", [128, 512], F32)
                tt = RR([sb(ph, f"ctt{i}", [128, 512], F32) for i in range(2)])
                stg = RR([sb(ph, f"cst{i}", [128, 512], BF16) for i in range(4)])
                seqs = [(CTX, SEQ, False), (0, CTX, False)] if l < DEPTH - 1 else [(CTX, HALF, True)]
                for (s0, Ls, rhalo) in seqs:
                    for g in range(4):
                        MS("pool", upad[:, g, 0:15], 0.0, [upad])
                        if rhalo:
                            DMA("sp", upad[:, g, 15:30 + Ls], U[g * 128:(g + 1) * 128, s0:s0 + Ls + 15], wr=[upad])
                        else:
                            MS("pool", upad[:, g, 15 + Ls:30 + Ls], 0.0, [upad])
                            DMA("sp", upad[:, g, 15:15 + Ls], U[g * 128:(g + 1) * 128, s0:s0 + Ls], wr=[upad])
                    for t in range(0, Ls, 512):
                        n = min(512, Ls - t)
                        for g in range(4):
                            for k in range(31):
                                MM(py[g][:, 0:n], dg[:, g, k, :], upad[:, g, t + k:t + k + n], k == 0, k == 30, [dg, upad], [py[g]])
                            ACT(yf[:, g, 0:n], py[g][:, 0:n], AF.Identity, [py[g], spt], [yf], bias=spc(l, "cdb", g), scale=1.0)
                            ACT(ysq[:, g, 0:n], py[g][:, 0:n], AF.Square, [py[g], spt], [ysq], bias=spc(l, "cdb", g), scale=1.0)
                            CP("pool", yb[:, g, 0:n], yf[:, g, 0:n], [yf], [yb])
                        for g in range(4):
                            MM(pm[:, 0:n], onesb[:], yb[:, g, 0:n], g == 0, g == 3, [onesb, yb], [pm])
                        for g in range(4):
                            MM(pv[:, 0:n], onesb[:], ysq[:, g, 0:n], g == 0, g == 3, [onesb, ysq], [pv])
                        ACT(mean[:, 0:n], pm[:, 0:n], AF.Identity, [pm], [mean], scale=1.0 / 512, bias=0.0)
                        TT_("dve", m2[:, 0:n], mean[:, 0:n], mean[:, 0:n], ALU.mult, [mean], [m2])
                        STT(var[:, 0:n], pv[:, 0:n], 1.0 / 512, m2[:, 0:n], ALU.mult, ALU.subtract, [pv, m2], [var])
                        ACT(var[:, 0:n], var[:, 0:n], AF.Ln, [var], [var], bias=EPS, scale=1.0)
                        ACT(var[:, 0:n], var[:, 0:n], AF.Exp, [var], [var], scale=-0.5)
                        for g in range(4):
                            t_ = tt(); st = stg()
                            TT_("dve", t_[:, 0:n], yf[:, g, 0:n], mean[:, 0:n], ALU.subtract, [yf, mean], [t_])
                            TT_("dve", t_[:, 0:n], t_[:, 0:n], var[:, 0:n], ALU.mult, [t_, var], [t_])
                            ACT(st[:, 0:n], t_[:, 0:n], AF.Silu, [t_, spt], [st], scale=spc(l, "clg", g), bias=spc(l, "clb", g))
                            DMA("sp", UC[g * 128:(g + 1) * 128, s0 + t:s0 + t + n], st[:, 0:n], rd=[st])
                S.barrier()

        def rnn_gen(l, ph):
            if True:
                rb = sb(ph, "rb", [128, 2, 2, 4, 128], BF16)
                DMA("pool", rb[:], rnnblk_d[l].rearrange("d w g k j -> k d w g j"), wr=[rb])
                hf = sb(ph, "hf", [128, TT], BF16)
                carry = sb(ph, "carry", [128, 1], F32)
                rdg = sb(ph, "rdg", [128, 8, 4, 128], BF16)
                for dgi_ in range(8):
                    for jj in range(4):
                        TS("dve", rdg[:, dgi_, jj, :], identb[:], spc(l, "rcw", dgi_ * 4 + jj), None, ALU.mult, None, [identb, spt], [rdg])
                pcv = RR([pst(ph, f"pcv{i}") for i in range(2)])
                NS = 2048
                def mk(i):
                    return dict(rxp=sb(ph, f"rxp{i}", [128, NS + 3], BF16), ucf=sb(ph, f"ucf{i}", [128, NS], F32), uc16=sb(ph, f"uc16{i}", [128, NS], BF16),
                                r=sb(ph, f"r{i}", [128, NS], F32), ig=sb(ph, f"ig{i}", [128, NS], F32), m=sb(ph, f"m{i}", [128, NS], F32),
                                bb=sb(ph, f"bb{i}", [128, NS], F32), hs=sb(ph, f"hs{i}", [128, NS], F32), rg=sb(ph, f"rg{i}", [128, NS], BF16),
                                st=sb(ph, f"rst{i}", [128, NS], BF16))
                sets = RR([mk(0), mk(1)])
                ppr = RR([pst(ph, f"ppr{i}") for i in range(3)]); ppi = RR([pst(ph, f"ppi{i}") for i in range(3)])
                NSEG = SEQ // NS
                lat = [(CTX + NS * i, NS) for i in range(NSEG)]
                steps = []
                for g in range(4):
                    for d in range(2):
                        half = (l == DEPTH - 1)
                        segs = [(0, CTX, True, True, not half)] + [(s0, n, i == 0, i == NSEG - 1, (not half) or i < NSEG // 2) for i, (s0, n) in enumerate(lat)]
                        if d == 1:
                            segs = [segs[0]] + segs[:0:-1]
                        elif half:
                            segs = segs[:1 + NSEG // 2]
                        for si, sg_ in enumerate(segs):
                            steps.append((g, d, si == 0) + sg_)
                def stageA(step):
                    g, d, first, s0, n, at_start, at_end, own = step
                    dgi = d * 4 + g
                    B = sets()
                    rxp, ucf, uc16, r, ig, m, rgt = (B[k] for k in ("rxp", "ucf", "uc16", "r", "ig", "m", "rg"))
                    rows = slice(g * 128, (g + 1) * 128)
                    if d == 0:
                        if at_start:
                            MS("pool", rxp[:, 0:3], 0.0, [rxp]); DMA("sp", rxp[:, 3:3 + n], RX[rows, s0:s0 + n], wr=[rxp])
                        else:
                            DMA("sp", rxp[:, 0:3 + n], RX[rows, s0 - 3:s0 + n], wr=[rxp])
                        offs = [0, 1, 2, 3]
                    else:
                        if at_end:
                            MS("pool", rxp[:, n:n + 3], 0.0, [rxp]); DMA("sp", rxp[:, 0:n], RX[rows, s0:s0 + n], wr=[rxp])
                        else:
                            DMA("sp", rxp[:, 0:n + 3], RX[rows, s0:s0 + n + 3], wr=[rxp])
                        offs = [3, 2, 1, 0]
                    if d == 1 and own:
                        DMA("sp", rgt[:, 0:n], RG[rows, s0:s0 + n], wr=[rgt])
                    for t in range(0, n, 512):
                        nn = min(512, n - t)
                        pc = pcv()
                        for jj in range(4):
                            MM(pc[:, 0:nn], rdg[:, dgi, jj, :], rxp[:, t + offs[jj]:t + offs[jj] + nn], jj == 0, jj == 3, [rdg, rxp], [pc])
                        ACT(ucf[:, t:t + nn], pc[:, 0:nn], AF.Identity, [pc, spt], [ucf], bias=spc(l, "rcb", dgi), scale=1.0)
                        CP("pool", uc16[:, t:t + nn], ucf[:, t:t + nn], [ucf], [uc16])
                    for t in range(0, n, 512):
                        nn = min(512, n - t)
                        pr = ppr(); pi = ppi()
                        MM(pr[:, 0:nn], rb[:, d, 0, g, :], uc16[:, t:t + nn], True, True, [rb, uc16], [pr])
                        MM(pi[:, 0:nn], rb[:, d, 1, g, :], uc16[:, t:t + nn], True, True, [rb, uc16], [pi])
                        ACT(r[:, t:t + nn], pr[:, 0:nn], AF.Sigmoid, [pr, spt], [r], bias=spc(l, "rba", dgi), scale=1.0)
                        ACT(ig[:, t:t + nn], pi[:, 0:nn], AF.Sigmoid, [pi, spt], [ig], bias=spc(l, "rbx", dgi), scale=1.0)
                    ACT(m[:, 0:n], r[:, 0:n], AF.Exp, [r, c16], [m], scale=c16[:, l, dgi:dgi + 1])
                    ACT(r[:, 0:n], r[:, 0:n], AF.Exp, [r, c8], [r], scale=c8[:, l, dgi:dgi + 1])
                    ACT(m[:, 0:n], m[:, 0:n], AF.Sqrt, [m], [m], scale=-1.0, bias=1.0)
                    return B
                def stageB(step, B):
                    g, d, first, s0, n, at_start, at_end, own = step
                    ucf, r, ig, m, bb, hs, rgt, st = (B[k] for k in ("ucf", "r", "ig", "m", "bb", "hs", "rg", "st"))
                    rows = slice(g * 128, (g + 1) * 128)
                    TT_("dve", bb[:, 0:n], ig[:, 0:n], ucf[:, 0:n], ALU.mult, [ig, ucf], [bb])
                    TT_("dve", bb[:, 0:n], bb[:, 0:n], m[:, 0:n], ALU.mult, [bb, m], [bb])
                    init = 0.0 if first else carry[:, 0:1]
                    if d == 0:
                        S.op("dve", "tensor_tensor_scan", dict(out=hs[:, 0:n], data0=r[:, 0:n], data1=bb[:, 0:n], initial=init, op0=ALU.mult, op1=ALU.add),
                             R([r, bb, carry]), R([hs]))
                        CP("dve", carry[:, 0:1], hs[:, n - 1:n], [hs], [carry])
                        if own:
                            CP("pool", hf[:, s0:s0 + n], hs[:, 0:n], [hs], [hf])
                    else:
                        S.op("dve", "tensor_tensor_scan", dict(out=hs[:, 0:n][:, ::-1], data0=r[:, 0:n][:, ::-1], data1=bb[:, 0:n][:, ::-1],
                             initial=init, op0=ALU.mult, op1=ALU.add), R([r, bb, carry]), R([hs]))
                        CP("dve", carry[:, 0:1], hs[:, 0:1], [hs], [carry])
                        if own:
                            TT_("pool", hs[:, 0:n], hs[:, 0:n], hf[:, s0:s0 + n], ALU.add, [hs, hf], [hs])
                            TT_("pool", st[:, 0:n], hs[:, 0:n], rgt[:, 0:n], ALU.mult, [hs, rgt], [st])
                            DMA("sp", RN[rows, s0:s0 + n], st[:, 0:n], rd=[st])
                yield len(steps)
                cur = stageA(steps[0])
                for si in range(len(steps)):
                    nxt_ = stageA(steps[si + 1]) if si + 1 < len(steps) else None
                    stageB(steps[si], cur)
                    cur = nxt_
                    yield 1

        def na_gen(l, ph):
            if True:
                def mkin(i):
                    return (sb(ph, f"kn{i}", [128, TT], BF16), [sb(ph, f"qz{i}_{h}", [128, TT], BF16) for h in range(2)],
                            sb(ph, f"vs{i}", [128, TT // 128, 130], BF16), sb(ph, f"nb{i}", [128, 2, ntypes, 128], BF16))
                insets = [mkin(0), mkin(1)]
                for (_, qz_, _, _) in insets:
                    for h in range(2): MS("pool", qz_[h][:], 0.0, [qz_[h]])
                def load_in(hg):
                    kn, qz, vs, nb = insets[hg % 2]
                    rows = slice(hg * 128, (hg + 1) * 128)
                    DMA("sp", kn[:], KN[rows, :], wr=[kn])
                    for h in range(2):
                        DMA("sp", qz[h][h * 64:(h + 1) * 64, :], QN[hg * 128 + h * 64:hg * 128 + (h + 1) * 64, :], wr=[qz[h]])
                    DMA("sp", vs[:], V.rearrange("(c p) f -> p c f", p=128)[:, :, hg * 130:(hg + 1) * 130], wr=[vs])
                    for h in range(2):
                        DMA("pool", nb[:, h], nab_d[l, 2 * hg + h].rearrange("t k q -> k t q"), wr=[nb])
                naT = sb(ph, "naT", [128, TT], BF16)
                psA = RR([pst(ph, f"psA{i}") for i in range(2)]); psB = RR([pst(ph, f"psB{i}") for i in range(2)])
                po = RR([pst(ph, f"po{i}") for i in range(2)])
                ptr = pst(ph, "ptr", (128, 1024), BF16)
                pT = RR([sb(ph, f"pT{i}", [128, 8, 128], BF16) for i in range(3)])
                nat = RR([sb(ph, f"nat{i}", [128, 128], BF16) for i in range(2)])
                rc = RR([sb(ph, f"rc{i}", [128, 2], F32) for i in range(2)])
                yield 4 * 2 * ((64 if l < DEPTH - 1 else HALF // 128) + (2 if l < DEPTH - 1 else 0))
                for hg in range(4):
                    rows = slice(hg * 128, (hg + 1) * 128)
                    kn, qz, vs, nb = insets[hg % 2]
                    if hg == 0: load_in(0)
                    if hg + 1 < 4: load_in(hg + 1)
                    units = [(CTX + 128 * p, [(2 + c, tid) for (c, tid) in naplan[p]] + [(0, None), (1, None)]) for p in range(64 if l < DEPTH - 1 else HALF // 128)]
                    if l < DEPTH - 1:
                        units += [(128 * pc, [(0, None), (1, None)]) for pc in range(2)]
                    items = [(ui, h) for ui in range(len(units)) for h in range(2)]
                    st_ = {}
                    def qk(idx):
                        ui, h = items[idx]; q0, chunks = units[ui]
                        pa = psA(); pb = psB(); pt = pT(); nch = len(chunks)
                        for i, (vc, tid) in enumerate(chunks):
                            bank = pa if i < 4 else pb
                            oap = bank[:, (i % 4) * 128:(i % 4 + 1) * 128]
                            MM(oap, kn[:, vc * 128:(vc + 1) * 128], qz[h][:, q0:q0 + 128], True, tid is None, [kn, qz[h]], [bank])
                            if tid is not None:
                                MM(oap, identb[:], nb[:, h, tid, :], False, True, [identb, nb], [bank])
                        na_ = min(nch, 4)
                        ACT(pt[:, 0:na_, :], pa[:, 0:na_ * 128].rearrange("p (c q) -> p c q", q=128), AF.Exp, [pa], [pt])
                        if nch > 4:
                            ACT(pt[:, 4:nch, :], pb[:, 0:(nch - 4) * 128].rearrange("p (c q) -> p c q", q=128), AF.Exp, [pb], [pt])
                        st_[idx] = pt
                    def pv(idx):
                        ui, h = items[idx]; q0, chunks = units[ui]; nch = len(chunks)
                        if h == 0:
                            st_["o"] = (po(), nat(), rc())
                        o_ps, nt, rc_ = st_["o"]; pt = st_.pop(idx)
                        for i, (vc, tid) in enumerate(chunks):
                            MM(o_ps[:, h * 65:(h + 1) * 65], pt[:, i, :], vs[:, vc, h * 65:(h + 1) * 65], i == 0, i == nch - 1, [pt, vs], [o_ps])
                        if h == 1:
                            S.op("dve", "reciprocal", dict(out=rc_[:, 0:2], in_=o_ps[:, 64:130:65]), R([o_ps]), R([rc_]))
                            for hh_ in range(2):
                                TS("dve", nt[:, hh_ * 64:(hh_ + 1) * 64], o_ps[:, hh_ * 65:hh_ * 65 + 64], rc_[:, hh_:hh_ + 1], None, ALU.mult, None, [o_ps, rc_], [nt])
                            S.op("pe", "transpose", dict(out=ptr[:, 0:128], in_=nt[:], identity=identb[:]), R([nt, identb]), R([ptr]))
                            ACT(naT[:, q0:q0 + 128], ptr[:, 0:128], AF.Identity, [ptr], [naT])
                    qk(0)
                    for idx in range(len(items)):
                        if idx + 1 < len(items): qk(idx + 1)
                        pv(idx)
                        yield 1
                    DMA("sp", NA[rows, :], naT[:], rd=[naT])

        def phase2_rnn_na(l):
            for gen in (rnn_gen, na_gen):
                with contextlib.ExitStack() as ph:
                    for _ in gen(l, ph): pass
                    S.barrier()

        def phase3a(l, Xsrc):
            with contextlib.ExitStack() as ph:
                pw = [sb(ph, f"pw{i}", [128, 4, D], BF16) for i in range(3)]
                for i, src in enumerate((conv_pw, na_out, rnn_out)):
                    DMA("pool", pw[i][:], src[l].rearrange("(k p) n -> p k n", p=128), wr=[pw[i]])
                ow = sb(ph, "ow", [128, 8, D], BF16)
                for s in range(2):
                    DMA("pool", ow[:, :, s * 512:(s + 1) * 512], out_w[l].rearrange("(k p) n -> p k n", p=128)[:, :, s * 512:(s + 1) * 512], wr=[ow])
                rw = sb(ph, "rw", [128, 8, NE], F32); rbias = sb(ph, "rbias", [128, 4 * NE], F32)
                DMA("sp", rw[:], router_w.rearrange("(k p) e -> p k e", p=128), wr=[rw]); DMA("sp", rbias[:], rbias_d[:, :], wr=[rbias])
                def mk(i):
                    return dict(br=[sb(ph, f"br{i}_{b}", [128, 4, 512], BF16) for b in range(3)], g3=sb(ph, f"g3{i}", [128, 24, 512], BF16),
                                x=sb(ph, f"x{i}", [128, 8, 512], F32))
                sets = RR([mk(0), mk(1)])
                mT = sb(ph, "mT", [128, 8, 512], BF16)
                pbr = RR([pst(ph, f"pb{b}") for b in range(5)])
                py = RR([pst(ph, f"p3y{i}") for i in range(2)])
                pss = pst(ph, "pss3"); prt = pss; ptg = pss
                tsr = RR([[sb(ph, f"t3_{i}_{b}", [128, 512], F32) for b in range(3)] for i in range(2)])
                sq = sb(ph, "sq3", [128, 8, 512], BF16); hn = sb(ph, "hn3", [128, 8, 512], F32); rs = sb(ph, "rs3", [128, 512], F32)
                h2o = sb(ph, "h2o", [128, 8, 512], BF16); gt = sb(ph, "gt3", [NE, 512], F32)
                hns = [S.res(f"hn{k}") for k in range(8)]
                def sm(name, w): return sb(ph, name, [128, w], F32)
                sc = sm("sc", 64); sl = sm("sl", 64); s2 = sm("s2", 64); msk = sm("msk", 64); wt = sm("wt", 64); gate = sm("gate", 64)
                m1 = sm("m1", 16); m2 = sm("m2", 16); gsc = sm("gsc", 16); goh = sm("goh", 16); gm = sm("gm", 4); ws = sm("ws", 4)
                def bc(ap, shape):
                    return ap.unsqueeze(2).broadcast_to(shape)
                mysubs = [s for s in SUBT if not (l == DEPTH - 1 and (s[2] == 1 or s[0] >= CTX + HALF))]
                def front(t0, n, j):
                    B = sets(); X_ = B["x"]
                    for b, src in enumerate((UC, NA, RN)):
                        DMA("sp", B["br"][b][:, :, 0:n], src[:, t0:t0 + n].rearrange("(g p) t -> p g t", p=128), wr=[B["br"][b]])
                    DMA("sp", B["g3"][:, :, 0:n], G3[:, t0:t0 + n].rearrange("(g p) t -> p g t", p=128), wr=[B["g3"]])
                    DMA("sp", X_[:, :, 0:n], Xsrc[:, t0:t0 + n].rearrange("(k p) t -> p k t", p=128), wr=[X_])
                    for og in range(8):
                        ts_ = tsr()
                        for b in range(3):
                            p_ = pbr()
                            for k in range(4):
                                MM(p_[:, 0:n], pw[b][:, k, og * 128:(og + 1) * 128], B["br"][b][:, k, 0:n], k == 0, k == 3, [pw[b], B["br"][b]], [p_])
                            TT_("dve", ts_[b][:, 0:n], p_[:, 0:n], B["g3"][:, b * 8 + og, 0:n], ALU.mult, [p_, B["g3"]], [ts_[b]])
                        TT_("pool", ts_[0][:, 0:n], ts_[0][:, 0:n], ts_[1][:, 0:n], ALU.add, [ts_[0], ts_[1]], [ts_[0]])
                        TT_("pool", mT[:, og, 0:n], ts_[0][:, 0:n], ts_[2][:, 0:n], ALU.add, [ts_[0], ts_[2]], [mT])
                    for og in range(8):
                        p_ = py()
                        for k in range(8):
                            MM(p_[:, 0:n], ow[:, k, og * 128:(og + 1) * 128], mT[:, k, 0:n], k == 0, k == 7, [ow, mT], [p_])
                        STT(X_[:, og, 0:n], p_[:, 0:n], modv[:, l, j, 16 + og:17 + og], X_[:, og, 0:n], ALU.mult, ALU.add, [p_, modv, X_], [X_])
                    DMA("sp", XM[:, t0:t0 + n].rearrange("(k p) t -> p k t", p=128), X_[:, :, 0:n], rd=[X_])
                    return X_
                def tail(t0, n, j, X_):
                    nb = n // 128
                    ACT(sq[:, :, 0:n], X_[:, :, 0:n], AF.Square, [X_], [sq])
                    for k in range(8):
                        MM(pss[:, 0:n], onesb[:], sq[:, k, 0:n], k == 0, k == 7, [onesb, sq], [pss])
                    rstd_from_ss(pss, n, rs, 1.0 / D)
                    for k in range(8):
                        TT_("dve", hn[:, k, 0:n], X_[:, k, 0:n], rs[:, 0:n], ALU.mult, [X_, rs], [hns[k]])
                        ACT(hn[:, k, 0:n], hn[:, k, 0:n], AF.Identity, [hns[k], gs2, modv], [hns[k]], scale=gs2[:, l, j, k:k + 1], bias=modv[:, l, j, 24 + k:25 + k])
                        CP("pool", h2o[:, k, 0:n], hn[:, k, 0:n], [hns[k]], [h2o])
                    DMA("sp", H2[:, t0:t0 + n].rearrange("(k p) t -> p k t", p=128), h2o[:, :, 0:n], rd=[h2o])
                    for tb in range(nb):
                        for k in range(8):
                            MM(prt[:, tb * NE:(tb + 1) * NE], hn[:, k, tb * 128:(tb + 1) * 128], rw[:, k, :], k == 0, k == 7, [hns[k], rw], [prt])
                    w = nb * NE; g4 = nb * 4
                    def v3(t_, inner): return t_[:, 0:w].rearrange("p (a e) -> p a e", e=inner)
                    ACT(sc[:, 0:w], prt[:, 0:w], AF.Sigmoid, [prt], [sc])
                    TT_("dve", sl[:, 0:w], sc[:, 0:w], rbias[:, 0:w], ALU.add, [sc, rbias], [sl])
                    S.op("dve", "tensor_reduce", dict(out=m1[:, 0:g4], in_=v3(sl, 4), axis=AX.X, op=ALU.max), R([sl]), R([m1]))
                    TT_("dve", v3(s2, 4), v3(sl, 4), bc(m1[:, 0:g4], [128, g4, 4]), ALU.is_equal, [sl, m1], [s2])
                    STT(s2[:, 0:w], s2[:, 0:w], -1e9, sl[:, 0:w], ALU.mult, ALU.add, [s2, sl], [s2])
                    S.op("dve", "tensor_reduce", dict(out=m2[:, 0:g4], in_=v3(s2, 4), axis=AX.X, op=ALU.max), R([s2]), R([m2]))
                    TT_("dve", gsc[:, 0:g4], m1[:, 0:g4], m2[:, 0:g4], ALU.add, [m1, m2], [gsc])
                    S.op("dve", "tensor_reduce", dict(out=gm[:, 0:nb], in_=gsc[:, 0:g4].rearrange("p (a e) -> p a e", e=4), axis=AX.X, op=ALU.max), R([gsc]), R([gm]))
                    TT_("dve", goh[:, 0:g4].rearrange("p (a e) -> p a e", e=4), gsc[:, 0:g4].rearrange("p (a e) -> p a e", e=4), bc(gm[:, 0:nb], [128, nb, 4]), ALU.is_equal, [gsc, gm], [goh])
                    TT_("dve", v3(msk, 4), v3(sl, 4), bc(m2[:, 0:g4], [128, g4, 4]), ALU.is_ge, [sl, m2], [msk])
                    TT_("dve", v3(msk, 4), v3(msk, 4), bc(goh[:, 0:g4], [128, g4, 4]), ALU.mult, [msk, goh], [msk])
                    TT_("dve", wt[:, 0:w], sc[:, 0:w], msk[:, 0:w], ALU.mult, [sc, msk], [wt])
                    S.op("dve", "tensor_reduce", dict(out=ws[:, 0:nb], in_=v3(wt, NE), axis=AX.X, op=ALU.add), R([wt]), R([ws]))
                    S.op("dve", "reciprocal", dict(out=ws[:, 0:nb], in_=ws[:, 0:nb]), R([ws]), R([ws]))
                    TT_("dve", v3(gate, NE), v3(wt, NE), bc(ws[:, 0:nb], [128, nb, NE]), ALU.mult, [wt, ws], [gate])
                    for tb in range(nb):
                        S.op("pe", "transpose", dict(out=ptg[0:NE, tb * 128:(tb + 1) * 128], in_=gate[:, tb * NE:(tb + 1) * NE], identity=ident[:]), R([gate, ident]), R([ptg]))
                    CP("dve", gt[:, 0:n], ptg[0:NE, 0:n], [ptg], [gt])
                    DMA("sp", GT[:, t0:t0 + n], gt[:, 0:n], rd=[gt])
                xs_ = front(*mysubs[0])
                for i_, s_ in enumerate(mysubs):
                    nx_ = front(*mysubs[i_ + 1]) if i_ + 1 < len(mysubs) else None
                    tail(*s_, xs_)
                    xs_ = nx_
                S.barrier()

        def phase3b(l, final):
            with contextlib.ExitStack() as ph:
                SMAX = 1280
                xm = sb(ph, "xm", [128, 8, SMAX], F32); xms = [S.res(f"xm{k}") for k in range(8)]
                hg = RR([(sb(ph, f"h2b{i}", [128, 8, SMAX], BF16), sb(ph, f"gT{i}", [NE, SMAX], F32)) for i in range(2)])
                selt = sb(ph, "selt", [NE, NE * 128], F32)
                DMA("sp", selt[:], sel_d[:, :], wr=[selt])
                wset = RR([(sb(ph, f"w1b{i}", [128, 8, DE], BF16), sb(ph, f"w3b{i}", [128, 8, DE], BF16), sb(ph, f"w2b{i}", [128, 4, D], BF16)) for i in range(2)])
                pg = RR([pst(ph, f"pg{i}") for i in range(2)])
                p1 = RR([pst(ph, f"p1_{i}") for i in range(2)]); p3 = RR([pst(ph, f"p3_{i}") for i in range(2)]); po = RR([pst(ph, f"po2{i}") for i in range(2)])
                gbs = RR([sb(ph, f"gb{i}", [128, 512], F32) for i in range(4)])
                s1 = RR([sb(ph, f"s1_{i}", [128, 512], F32) for i in range(2)]); tt = RR([sb(ph, f"tt_{i}", [128, 512], F32) for i in range(2)])
                he = RR([sb(ph, f"he{i}", [128, 4, 512], BF16) for i in range(2)])
                subs = [s for s in SUBT if not (l == DEPTH - 1 and (s[2] == 1 or s[0] >= CTX + HALF))]
                supers = [subs[0:3]] + [subs[i:i + 2] for i in range(3, len(subs), 2)] if l < DEPTH - 1 else [subs[i:i + 2] for i in range(0, len(subs), 2)]
                def load_hg(sup):
                    c0 = sup[0][0]; Sn = sum(s[1] for s in sup); h2b, gT = hg()
                    DMA("sp", h2b[:, :, 0:Sn], H2[:, c0:c0 + Sn].rearrange("(k p) t -> p k t", p=128), wr=[h2b])
                    DMA("sp", gT[:, 0:Sn], GT[:, c0:c0 + Sn], wr=[gT])
                    return h2b, gT
                nxt_hg = load_hg(supers[0])
                wcache = {}
                def weights(key, e):
                    if key not in wcache:
                        w1b, w3b, w2b = wset()
                        for half in range(2):
                            DMA("pool", w1b[:, half * 4:(half + 1) * 4, :], w1_d[l, e].rearrange("(k p) n -> p k n", p=128)[:, half * 4:(half + 1) * 4, :], wr=[w1b])
                            DMA("pool", w3b[:, half * 4:(half + 1) * 4, :], w3_d[l, e].rearrange("(k p) n -> p k n", p=128)[:, half * 4:(half + 1) * 4, :], wr=[w3b])
                            DMA("pool", w2b[:, half * 2:(half + 1) * 2, :], w2_d[l, e].rearrange("(k p) n -> p k n", p=128)[:, half * 2:(half + 1) * 2, :], wr=[w2b])
                        wcache[key] = (w1b, w3b, w2b)
                    return wcache[key]
                for si, sup in enumerate(supers):
                    c0 = sup[0][0]; Sn = sum(s[1] for s in sup)
                    h2b, gT = nxt_hg
                    for og in range(8):
                        S.dma("sp", xm[:, og, 0:Sn], XM[og * 128:(og + 1) * 128, c0:c0 + Sn], [], [xms[og]])
                    ulist = [(e, s_) for e in range(NE) for s_ in sup]
                    def gate_bc(ui):
                        e, (t0, n, j) = ulist[ui]; g_ = gbs()
                        DMA("sp", g_[:, 0:n], GT[e:e + 1, t0:t0 + n].broadcast_to([128, n]), wr=[g_])
                        return g_
                    def down(ui, he_):
                        e, (t0, n, j) = ulist[ui]; o = t0 - c0; w2b = weights((si, e), e)[2]
                        for og in range(8):
                            p_ = po()
                            for k in range(4):
                                MM(p_[:, 0:n], w2b[:, k, og * 128:(og + 1) * 128], he_[:, k, 0:n], k == 0, k == 3, [w2b, he_], [p_])
                            S.op("dve", "scalar_tensor_tensor", dict(out=xm[:, og, o:o + n], in0=p_[:, 0:n], scalar=modv[:, l, j, 40 + og:41 + og], in1=xm[:, og, o:o + n],
                                 op0=ALU.mult, op1=ALU.add), [p_.res, modv.res, xms[og]], [xms[og]])
                    gq = [gate_bc(0)] + ([gate_bc(1)] if len(ulist) > 1 else []); prev = None
                    for ui, (e, (t0, n, j)) in enumerate(ulist):
                        o = t0 - c0; w1b, w3b, w2b = weights((si, e), e)
                        he_ = he()
                        for fc in range(4):
                            p1_ = p1(); p3_ = p3()
                            for k in range(8):
                                MM(p1_[:, 0:n], w1b[:, k, fc * 128:(fc + 1) * 128], h2b[:, k, o:o + n], k == 0, k == 7, [w1b, h2b], [p1_])
                            for k in range(8):
                                MM(p3_[:, 0:n], w3b[:, k, fc * 128:(fc + 1) * 128], h2b[:, k, o:o + n], k == 0, k == 7, [w3b, h2b], [p3_])
                            s1_ = s1(); t_ = tt()
                            ACT(s1_[:, 0:n], p1_[:, 0:n], AF.Silu, [p1_], [s1_])
                            TT_("dve", t_[:, 0:n], s1_[:, 0:n], p3_[:, 0:n], ALU.mult, [s1_, p3_], [t_])
                            TT_("pool", he_[:, fc, 0:n], t_[:, 0:n], gq[0][:, 0:n], ALU.mult, [t_, gq[0]], [he_])
                        gq.pop(0)
                        if ui + 2 < len(ulist):
                            gq.append(gate_bc(ui + 2))
                        if prev is not None: down(*prev)
                        prev = (ui, he_)
                        if (ui == 0 or ulist[ui - 1][0] != e):
                            if e + 1 < NE:
                                weights((si, e + 1), e + 1)
                            elif si + 1 < len(supers):
                                weights((si + 1, 0), 0)
                        if ui == 0 and si + 1 < len(supers):
                            nxt_hg = load_hg(supers[si + 1])
                    down(*prev)
                    dst = outT[:, c0 - CTX:c0 - CTX + Sn] if final else XN[:, c0:c0 + Sn]
                    for og in range(8):
                        S.dma("sp", dst[og * 128:(og + 1) * 128, :], xm[:, og, 0:Sn], [xms[og]], [])
                S.barrier()

        order = ["p1", "conv", "rnn", "na", "p3a", "p3b"]
        def run_layer(l, Xsrc, stop=None):
            for name, fn in (("p1", lambda: (phase1(l, Xsrc, preW0 if l == 0 else None), w0stack.close() if l == 0 else None)), ("conv", lambda: phase2_conv(l)), ("na", lambda: phase2_rnn_na(l)), ("p3a", lambda: phase3a(l, Xsrc)), ("p3b", lambda: phase3b(l, l == DEPTH - 1))):
                fn()
                if stop == name:
                    return True
            return False
        if upto == "all":
            run_layer(0, xT); run_layer(1, XN)
        elif upto.startswith("only:"):
            {"conv": lambda: phase2_conv(0), "na": lambda: phase2_rnn_na(0)}[upto[5:]]()
        elif upto == "l0":
            run_layer(0, xT)
        else:
            run_layer(0, xT, stop=upto)
        for n_, ap in dump_out.items():
            S.dma("sp", ap, scr[n_])
        S.barrier()
        S.emit()
    return nc


def prep_shared(inp, rev=False):
    types, _ = na_geometry()
    sh = {}
    dsl = slice(None, None, -1) if rev else slice(None)
    sh["ident_in"] = np.eye(128, dtype=np.float32)
    sh["sp"] = np.stack([pack_small(inp, l, rev) for l in range(DEPTH)])
    sh["inbv"] = np.stack([np.ascontiguousarray(np.broadcast_to(inp["in_b"][l, O_V:O_V + 512], (128, 512))) for l in range(DEPTH)])
    sh["rbias"] = np.ascontiguousarray(np.broadcast_to(np.tile(inp["router_bias"], 4), (128, 4 * NE)))
    for k in ("mod_w", "in_w", "conv_pw_w", "na_out_w", "rnn_out_w", "out_w", "exp_w1", "exp_w3", "exp_w2", "router_w"):
        sh[k] = np.ascontiguousarray(inp[k], dtype=np.float32)
    blk = np.zeros((DEPTH, 2, 2, 4, 128, 128), np.float32)
    for l in range(DEPTH):
        for d in range(2):
            for wi, wn in enumerate(("rnn_wa", "rnn_wx")):
                w = inp[wn][l][dsl][d]
                for n in range(8):
                    g, o = n // 2, (n % 2) * 64
                    blk[l, d, wi, g, o:o + 64, o:o + 64] = w[n]
    sh["rnnblk"] = blk
    sh["nabias"] = np.stack([build_na_bias(inp["na_rpb"][l], rev) for l in range(DEPTH)])
    sel = np.zeros((NE, NE * 128), np.float32)
    for e in range(NE):
        sel[e, e * 128:(e + 1) * 128] = 1.0
    sh["sel"] = sel
    return sh


def prep_core(inp, b, rev=False):
    m = {}
    dsl = slice(None, None, -1) if rev else slice(None)
    m["xT"] = np.ascontiguousarray(np.concatenate([inp["ctx"][b][dsl].T, inp["x"][b][dsl].T], axis=1), dtype=np.float32)
    cp = np.stack([_fm(inp["c"][b]), _fm(inp["c_ctx"])], axis=-1)
    m["c_pk"] = np.ascontiguousarray(cp.reshape(128, 16), dtype=np.float32)
    return m


_NC_CACHE = {}


def kernel(**inputs):
    inp = {k: np.asarray(v) for k, v in inputs.items()}
    if "full" not in _NC_CACHE:
        _NC_CACHE["full"] = build()
    nc = _NC_CACHE["full"]
    shs = [prep_shared(inp, False), prep_shared(inp, True)]
    in_maps = []
    for core in range(8):
        rev = core >= 4
        m = dict(shs[int(rev)]); m.update(prep_core(inp, core % 4, rev)); in_maps.append(m)
    res = run_bass_kernel_spmd(nc, in_maps, core_ids=list(range(8)))
    out = np.empty((4, SEQ, D), np.float32)
    for b in range(4):
        out[b, :HALF] = res.results[b]["outT"].T
        out[b, HALF:] = res.results[b + 4]["outT"].T[::-1]
    return out
```

```python
import contextlib
import numpy as np
import concourse.bass as bass
import concourse.mybir as mybir
from concourse.bass_utils import run_bass_kernel_spmd

F32 = mybir.dt.float32
BF16 = mybir.dt.bfloat16
AF = mybir.ActivationFunctionType
ALU = mybir.AluOpType
AX = mybir.AxisListType

D = 1024; SEQ = 8192; CTX = 256; TT = SEQ + CTX; DEPTH = 2
DIN = 6656; NE = 16; DE = 512
EPS = 1e-6
NEG = -30000.0
O_A, O_GA, O_Q, O_K, O_V, O_RX, O_RG, O_GCV, O_GNA, O_GRN = 0, 512, 1024, 1536, 2048, 2560, 3072, 3584, 4608, 5632

SP_FIELDS = [("mod_b", 48), ("n1g", 8), ("n2g", 8), ("in_b", 52), ("cdw", 124), ("cdb", 4), ("clg", 4), ("clb", 4),
             ("qg", 1), ("kg", 1), ("rcw", 32), ("rcb", 8), ("rba", 8), ("rbx", 8), ("lam", 8)]
SP_OFF = {}
_o = 0
for _n, _w in SP_FIELDS:
    SP_OFF[_n] = (_o, _w); _o += _w
SP_W = _o


def _fm(v):
    return np.ascontiguousarray(v.reshape(-1, 128).T)


def pack_small(inp, l, rev=False):
    sp = np.zeros((128, SP_W), np.float32)
    dsl = slice(None, None, -1) if rev else slice(None)
    def put(name, arr):
        o, w = SP_OFF[name]; assert arr.shape == (128, w), (name, arr.shape); sp[:, o:o + w] = arr
    put("mod_b", _fm(inp["mod_b"][l])); put("n1g", _fm(inp["norm1_g"][l])); put("n2g", _fm(inp["norm2_g"][l]))
    put("in_b", _fm(inp["in_b"][l]))
    cw = inp["conv_dw_w"][l][dsl]
    put("cdw", np.ascontiguousarray(cw.T.reshape(4, 128, 31).transpose(1, 0, 2)).reshape(128, 124))
    put("cdb", _fm(inp["conv_dw_b"][l])); put("clg", _fm(inp["conv_ln_g"][l])); put("clb", _fm(inp["conv_ln_b"][l]))
    put("qg", np.tile(inp["na_q_g"][l], 2)[:, None]); put("kg", np.tile(inp["na_k_g"][l], 2)[:, None])
    rw = inp["rnn_conv_w"][l][dsl]
    put("rcw", np.ascontiguousarray(rw.transpose(2, 0, 1).reshape(4, 128, 2, 4).transpose(1, 2, 0, 3)).reshape(128, 32))
    def dg(a):
        return np.ascontiguousarray(a.reshape(2, 4, 128).transpose(2, 0, 1)).reshape(128, 8)
    put("rcb", dg(inp["rnn_conv_b"][l][dsl])); put("rba", dg(inp["rnn_ba"][l][dsl])); put("rbx", dg(inp["rnn_bx"][l][dsl]))
    put("lam", dg(inp["rnn_lam"][l][dsl]))
    return sp


def na_geometry():
    types = {}; plan = []
    for p in range(64):
        rs0 = min(max(2 * p - 4, 0), 120); rs1 = min(max(2 * p + 1 - 4, 0), 120)
        lo = min(rs0, rs1); hi = max(rs0, rs1) + 7
        lst = []
        for c in range(lo // 2, hi // 2 + 1):
            key = (c - p, rs0 - 2 * p, rs1 - (2 * p + 1))
            if key not in types:
                types[key] = len(types)
            lst.append((c, types[key]))
        plan.append(lst)
    return types, plan


def na_true_tile(rpb, p, c):
    k = np.arange(128)[:, None]; q = np.arange(128)[None, :]
    krow = 2 * c + k // 64; kc = k % 64; qrow = 2 * p + q // 64; qc = q % 64
    rs = np.clip(qrow - 4, 0, 120); cs = np.clip(qc - 8, 0, 48)
    valid = (krow >= rs) & (krow < rs + 8) & (kc >= cs) & (kc < cs + 16)
    dr = np.clip(krow - qrow + 7, 0, 14); dc = np.clip(kc - qc + 15, 0, 30)
    val = rpb[:, dr, dc]
    return np.where(valid[None], val, np.float32(NEG)).astype(np.float32)


def build_na_bias(rpb, rev=False):
    types, plan = na_geometry()
    H = rpb.shape[0]
    out = np.full((H, len(types), 128, 128), NEG, np.float32)
    done = set()
    for p in range(64):
        for (c, tid) in plan[p]:
            if tid in done: continue
            done.add(tid)
            out[:, tid] = na_true_tile(rpb, 63 - p, 63 - c)[:, ::-1, ::-1] if rev else na_true_tile(rpb, p, c)
    return out


class Res:
    __slots__ = ("name", "w", "r")
    def __init__(self, name):
        self.name = name; self.w = None; self.r = {}


class Tl:
    __slots__ = ("t", "res")
    def __init__(self, t, res):
        self.t = t; self.res = res
    def __getitem__(self, idx):
        return self.t[idx]


class Sched:
    SEM_LIMIT = 30000
    def __init__(self, nc, stack, n_sems=84, dma_pool=10):
        self.nc = nc
        self.engs = {"pe": nc.tensor, "dve": nc.vector, "act": nc.scalar, "pool": nc.gpsimd, "sp": nc.sync}
        self.free_sems = [stack.enter_context(nc.semaphore(f"s{i}")) for i in range(n_sems)]
        self.q = {k: [] for k in self.engs}
        self.sem = {k: self.free_sems.pop() for k in self.engs}
        self.cnt = {k: 0 for k in self.engs}
        self.waited = {k: {} for k in self.engs}
        self.pend = {k: ([], []) for k in self.engs}
        self.dma_sems = {k: [[self.free_sems.pop(), 0] for _ in range(dma_pool)] for k in ("sp", "pool", "act")}
        self.dma_rr = {k: 0 for k in self.dma_sems}
        self.last_ev = {}
        self.nres = 0

    def res(self, name=None):
        self.nres += 1
        return Res(name or f"r{self.nres}")

    def _wait(self, eng, ev):
        sem, val = ev
        key = id(sem)
        if self.waited[eng].get(key, 0) >= val:
            return
        self.waited[eng][key] = val
        self.q[eng].append(lambda e, sem=sem, val=val: e.wait_ge(sem, val))

    def _deps(self, eng, reads, writes, skip_same):
        deps = {}
        def add(ev):
            if ev is None: return
            k = id(ev[0])
            if k not in deps or deps[k][1] < ev[1]: deps[k] = ev
        for r in reads: add(r.w)
        for w in writes:
            add(w.w)
            for ev in w.r.values(): add(ev)
        own = id(self.sem[eng])
        for k, ev in deps.items():
            if skip_same and k == own: continue
            self._wait(eng, ev)

    def _commit(self, ev, reads, writes):
        for w in writes:
            w.w = ev; w.r = {}
        for r in reads:
            k = id(ev[0])
            if k not in r.r or r.r[k][1] < ev[1]: r.r[k] = ev

    def op(self, eng, meth, kw, reads=(), writes=(), sig=True):
        self._deps(eng, reads, writes, skip_same=(eng == "pe"))
        fn = lambda e, meth=meth, kw=kw: getattr(e, meth)(**kw)
        if not sig:
            self.pend[eng][0].extend(reads); self.pend[eng][1].extend(writes)
            self.q[eng].append(lambda e, fn=fn: fn(e))
            return None
        if self.cnt[eng] >= self.SEM_LIMIT:
            self.sem[eng] = self.free_sems.pop(); self.cnt[eng] = 0
        self.cnt[eng] += 1
        sem = self.sem[eng]; ev = (sem, self.cnt[eng])
        self.q[eng].append(lambda e, fn=fn, sem=sem: fn(e).then_inc(sem, 1))
        pr, pw = self.pend[eng]
        self._commit(ev, list(reads) + pr, list(writes) + pw)
        self.pend[eng] = ([], [])
        self.last_ev[eng] = ev
        return ev

    def dma(self, qn, out, in_, reads=(), writes=()):
        self._deps(qn, reads, writes, skip_same=False)
        slot = self.dma_sems[qn][self.dma_rr[qn] % len(self.dma_sems[qn])]
        self.dma_rr[qn] += 1
        if slot[1] >= self.SEM_LIMIT:
            slot[0] = self.free_sems.pop(); slot[1] = 0
        if slot[1] > 0:
            self._wait(qn, (slot[0], slot[1]))
        slot[1] += 16
        sem = slot[0]; ev = (sem, slot[1])
        self.q[qn].append(lambda e, out=out, in_=in_, sem=sem: e.dma_start(out=out, in_=in_).then_inc(sem, 16))
        self._commit(ev, reads, writes)
        return ev

    def barrier(self):
        evs = [ev for ev in self.last_ev.values()]
        for qn, slots in self.dma_sems.items():
            for s in slots:
                if s[1] > 0: evs.append((s[0], s[1]))
        for eng in self.engs:
            for ev in evs:
                if id(ev[0]) == id(self.sem[eng]) and eng == "pe": continue
                self._wait(eng, ev)

    def emit(self):
        with self.nc.Block() as block:
            for name, attr in (("pe", "tensor"), ("dve", "vector"), ("act", "scalar"), ("pool", "gpsimd"), ("sp", "sync")):
                q = self.q[name]
                def body(e, q=q):
                    for f in q: f(e)
                getattr(block, attr)(body)


SUBT = [(0, CTX, 1)] + [(CTX + 512 * i, 512, 0) for i in range(16)]
HALF = SEQ // 2; NOWN = HALF // 512
VW = 8 * 65


def build(upto="all", dump=()):
    nc = bass.Bass("TRN2", target_bir_lowering=False)
    def din(name, shape, dt=F32):
        return nc.dram_tensor(name, list(shape), dt, kind="ExternalInput").ap()
    def dscr(name, shape, dt):
        return nc.dram_tensor(name, list(shape), dt, kind="Internal").ap()
    xT = din("xT", [D, TT]); c_pk = din("c_pk", [128, 16]); ident_d = din("ident_in", [128, 128])
    sp_d = din("sp", [DEPTH, 128, SP_W]); inbv_d = din("inbv", [DEPTH, 128, 512]); rbias_d = din("rbias", [128, 4 * NE])
    mod_w = din("mod_w", [DEPTH, D, 6 * D]); in_w = din("in_w", [DEPTH, D, DIN])
    conv_pw = din("conv_pw_w", [DEPTH, 512, D]); na_out = din("na_out_w", [DEPTH, 512, D])
    rnn_out = din("rnn_out_w", [DEPTH, 512, D]); out_w = din("out_w", [DEPTH, D, D])
    w1_d = din("exp_w1", [DEPTH, NE, D, DE]); w3_d = din("exp_w3", [DEPTH, NE, D, DE]); w2_d = din("exp_w2", [DEPTH, NE, DE, D])
    router_w = din("router_w", [D, NE]); rnnblk_d = din("rnnblk", [DEPTH, 2, 2, 4, 128, 128])
    natypes, naplan = na_geometry(); ntypes = len(natypes)
    nab_d = din("nabias", [DEPTH, 8, ntypes, 128, 128]); sel_d = din("sel", [NE, NE * 128])
    outT = nc.dram_tensor("outT", [D, HALF], F32, kind="ExternalOutput").ap()
    U = dscr("U", [512, TT], BF16); QN = dscr("QN", [512, TT], BF16); KN = dscr("KN", [512, TT], BF16)
    V = dscr("V", [TT, VW], BF16); RX = dscr("RX", [512, TT], BF16); RG = dscr("RG", [512, TT], BF16)
    G3 = dscr("G3", [3 * D, TT], BF16); UC = dscr("UC", [512, TT], BF16); NA = dscr("NA", [512, TT], BF16)
    RN = dscr("RN", [512, TT], BF16); XM = dscr("XM", [D, TT], F32); XN = dscr("XN", [D, TT], F32)
    H2 = dscr("H2", [D, TT], BF16); GT = dscr("GT", [NE, TT], F32)
    scr = dict(H2=H2, GT=GT, U=U, QN=QN, KN=KN, V=V, RX=RX, RG=RG, G3=G3, UC=UC, NA=NA, RN=RN, XM=XM, XN=XN)
    dump_out = {n: nc.dram_tensor("dump_" + n, list(scr[n].shape), scr[n].dtype, kind="ExternalOutput").ap() for n in dump}

    with contextlib.ExitStack() as top:
        S = Sched(nc, top)
        uid = [0]
        def sb(stack, name, shape, dt):
            uid[0] += 1
            return Tl(stack.enter_context(nc.sbuf_tensor(f"{name}_{uid[0]}", list(shape), dt)), S.res(name))
        def pst(stack, name, shape=(128, 512), dt=F32):
            uid[0] += 1
            return Tl(stack.enter_context(nc.psum_tensor(f"{name}_{uid[0]}", list(shape), dt)), S.res(name))
        def R(ts): return [t.res if hasattr(t, 'res') else t for t in ts]
        def ACT(out, in_, func, rd, wr, **kw):
            S.op("act", "activation", dict(out=out, in_=in_, func=func, **kw), R(rd), R(wr))
        def MM(out, lhsT, rhs, start, stop, rd, wr):
            S.op("pe", "matmul", dict(out=out, lhsT=lhsT, rhs=rhs, start=start, stop=stop), R(rd), R(wr), sig=stop)
        def TT_(eng, out, in0, in1, op, rd, wr):
            S.op(eng, "tensor_tensor", dict(out=out, in0=in0, in1=in1, op=op), R(rd), R(wr))
        def STT(out, in0, scalar, in1, op0, op1, rd, wr):
            S.op("dve", "scalar_tensor_tensor", dict(out=out, in0=in0, scalar=scalar, in1=in1, op0=op0, op1=op1), R(rd), R(wr))
        def TS(eng, out, in0, s1, s2, op0, op1, rd, wr):
            kw = dict(out=out, in0=in0, scalar1=s1, scalar2=s2, op0=op0)
            if op1 is not None: kw["op1"] = op1
            S.op(eng, "tensor_scalar", kw, R(rd), R(wr))
        def CP(eng, out, in_, rd, wr):
            S.op(eng, "tensor_copy", dict(out=out, in_=in_), R(rd), R(wr))
        def MS(eng, ap, val, wr):
            S.op(eng, "memset", dict(ap=ap, constant=val), [], R(wr))
        def DMA(q, out, in_, rd=(), wr=()):
            S.dma(q, out, in_, R(rd), R(wr))
        class RR:
            def __init__(self, lst): self.l = lst; self.i = -1
            def __call__(self):
                self.i += 1; return self.l[self.i % len(self.l)]

        ident = sb(top, "ident", [128, 128], F32); identb = sb(top, "identb", [128, 128], BF16)
        onesb = sb(top, "onesb", [128, 128], BF16)
        blk64 = sb(top, "blk64", [128, 128], BF16)
        spt = sb(top, "spt", [128, DEPTH, SP_W], F32)
        cact = sb(top, "cact", [128, 8, 2], F32)
        modv = sb(top, "modv", [128, DEPTH, 2, 48], F32)
        gs1 = sb(top, "gs1", [128, DEPTH, 2, 8], F32); gs2 = sb(top, "gs2", [128, DEPTH, 2, 8], F32)
        c8 = sb(top, "c8", [128, DEPTH, 8], F32); c16 = sb(top, "c16", [128, DEPTH, 8], F32)
        def spc(l, name, i=0, w=1):
            o, _ = SP_OFF[name]; return spt[:, l, o + i:o + i + w]

        def load_W(l, stack):
            W = sb(stack, "W", [128, 8, DIN], BF16)
            Ws = [S.res(f"W{s}") for s in range(13)]
            order = list(range(13)) if l < DEPTH - 1 else [5, 3, 4, 0, 1, 2, 6, 7, 8, 9, 10, 11, 12]
            for s in order:
                DMA("pool", W[:, :, s * 512:(s + 1) * 512], in_w[l].rearrange("(k p) n -> p k n", p=128)[:, :, s * 512:(s + 1) * 512], wr=[Ws[s]])
            return W, Ws

        w0stack = top.enter_context(contextlib.ExitStack())
        preW0 = load_W(0, w0stack)
        with contextlib.ExitStack() as ph:
            ps0 = pst(ph, "ps0")
            mwb = RR([sb(ph, f"mwb{i}", [128, 8, 512], F32) for i in range(2)])
            ctmp = sb(ph, "ctmp", [128, 16], F32)
            DMA("sp", ident[:], ident_d[:, :], wr=[ident])
            DMA("sp", spt[:], sp_d.rearrange("l p w -> p l w"), wr=[spt])
            DMA("sp", ctmp[:], c_pk[:, :], wr=[ctmp])
            CP("dve", identb[:], ident[:], [ident], [identb])
            MS("dve", onesb[:], 1.0, [onesb]); MS("dve", blk64[:], 0.0, [blk64])
            MS("dve", blk64[0:64, 0:64], 1.0, [blk64]); MS("dve", blk64[64:128, 64:128], 1.0, [blk64])
            ACT(cact[:].rearrange("p k j -> p (k j)"), ctmp[:], AF.Silu, [ctmp], [cact])
            for l in range(DEPTH):
                ACT(c8[:, l, :], spc(l, "lam", 0, 8), AF.Exp, [spt], [c8], scale=-1.0)
                ACT(c8[:, l, :], c8[:, l, :], AF.Ln, [c8], [c8], bias=1.0, scale=1.0)
                TS("dve", c16[:, l, :], c8[:, l, :], -16.0, None, ALU.mult, None, [c8], [c16])
                TS("dve", c8[:, l, :], c8[:, l, :], -8.0, None, ALU.mult, None, [c8, c16], [c8])
            for l in range(DEPTH):
                for s in range(12):
                    buf = mwb()
                    DMA("sp", buf[:], mod_w[l].rearrange("(k p) n -> p k n", p=128)[:, :, s * 512:(s + 1) * 512], wr=[buf])
                    for g in range(4):
                        gg = s * 4 + g
                        for k in range(8):
                            MM(ps0[:, 2 * gg:2 * gg + 2], buf[:, k, g * 128:(g + 1) * 128], cact[:, k, :], k == 0, k == 7, [buf, cact], [ps0])
                for j in range(2):
                    TT_("dve", modv[:, l, j, :], ps0[:, j:96:2], spc(l, "mod_b", 0, 48), ALU.add, [ps0, spt], [modv])
                    STT(gs1[:, l, j, :], modv[:, l, j, 8:16], 1.0, spc(l, "n1g", 0, 8), ALU.add, ALU.mult, [modv, spt], [gs1])
                    STT(gs2[:, l, j, :], modv[:, l, j, 32:40], 1.0, spc(l, "n2g", 0, 8), ALU.add, ALU.mult, [modv, spt], [gs2])
            S.barrier()

        def rstd_from_ss(ss_ps, n, rs, inv_n):
            ACT(rs[:, 0:n], ss_ps[:, 0:n], AF.Ln, [ss_ps], [rs], scale=inv_n, bias=EPS)
            ACT(rs[:, 0:n], rs[:, 0:n], AF.Exp, [rs], [rs], scale=-0.5)

        def phase1(l, Xsrc, pre=None):
            with contextlib.ExitStack() as ph:
                W, Ws = pre if pre is not None else load_W(l, ph)
                inbv = sb(ph, "inbv", [128, 512], F32)
                DMA("sp", inbv[:], inbv_d[l], wr=[inbv])
                X = sb(ph, "X", [128, 8, 512], F32); sq = sb(ph, "sq", [128, 8, 512], BF16)
                rs = sb(ph, "rs", [128, 512], F32); hh = [sb(ph, f"h{i}", [128, 8, 512], BF16) for i in range(2)]
                hres = {id(hh[b_]): [S.res(f"h{b_}_{k}") for k in range(8)] for b_ in range(2)}
                pss = pst(ph, "pss"); pq = pst(ph, "pq")
                pz = RR([pst(ph, f"pz{i}") for i in range(5)])
                stg = RR([sb(ph, f"stg{i}", [128, 512], BF16) for i in range(6)])
                vst = RR([sb(ph, f"vst{i}", [128, 8, 65], BF16) for i in range(2)])
                for t in vst.l: MS("dve", t[:], 1.0, [t])
                qset = RR([(sb(ph, f"qb{i}", [128, 512], F32), sb(ph, f"qs{i}", [128, 512], BF16), sb(ph, f"qr{i}", [128, 512], F32)) for i in range(4)])
                sg = RR([sb(ph, f"sg{i}", [128, 512], F32) for i in range(2)])
                def inb(g): return spc(l, "in_b", g, 1)
                Xs = [S.res(f"X{k}") for k in range(8)]
                def loadX(i):
                    t0, n, j = SUBT[i]
                    S.dma("sp", X[:, :, 0:n], Xsrc[:, t0:t0 + n].rearrange("(k p) t -> p k t", p=128), [], Xs)
                def square(i):
                    t0, n, j = SUBT[i]
                    S.op("act", "activation", dict(out=sq[:, :, 0:n], in_=X[:, :, 0:n], func=AF.Square), Xs, [sq.res])
                def prologue(i, h):
                    t0, n, j = SUBT[i]
                    for k in range(8):
                        MM(pss[:, 0:n], onesb[:], sq[:, k, 0:n], k == 0, k == 7, [onesb, sq], [pss])
                    rstd_from_ss(pss, n, rs, 1.0 / D)
                    for k in range(8):
                        S.op("dve", "tensor_tensor", dict(out=X[:, k, 0:n], in0=X[:, k, 0:n], in1=rs[:, 0:n], op=ALU.mult), [Xs[k], rs.res], [Xs[k]])
                        S.op("act", "activation", dict(out=h[:, k, 0:n], in_=X[:, k, 0:n], func=AF.Identity, scale=gs1[:, l, j, k:k + 1], bias=modv[:, l, j, k:k + 1]),
                             [Xs[k], gs1.res, modv.res], [hres[id(h)][k]])
                NT = len(SUBT)
                ALLW = ("U", "Q", "K", "V", "RX", "RG", "G3")
                def wants(i):
                    if l < DEPTH - 1: return ALLW
                    if i == 0: return ("K", "V", "RX")
                    if i <= NOWN: return ALLW
                    if i == NOWN + 1: return ("U", "K", "V", "RX")
                    return ("RX",)
                def main(i, h, sq_next):
                    t0, n, j = SUBT[i]; want = wants(i)
                    def zmm(ps, col0, n):
                        for k in range(8):
                            MM(ps[:, 0:n], W[:, k, col0:col0 + 128], h[:, k, 0:n], k == 0, k == 7, [Ws[col0 // 512], hres[id(h)][k]], [ps])
                    for g in range(4 if "U" in want else 0):
                        pa = pz(); zmm(pa, O_A + g * 128, n)
                        pg = pz(); zmm(pg, O_GA + g * 128, n)
                        s_ = sg(); st = stg()
                        ACT(s_[:, 0:n], pg[:, 0:n], AF.Sigmoid, [pg, spt], [s_], bias=inb(4 + g), scale=1.0)
                        STT(st[:, 0:n], pa[:, 0:n], inb(g), s_[:, 0:n], ALU.add, ALU.mult, [pa, s_, spt], [st])
                        DMA("sp", U[g * 128:(g + 1) * 128, t0:t0 + n], st[:, 0:n], rd=[st])
                    pend = []
                    def finish(b_, s2, r_, st, gname, extra, dst, g):
                        MM(pq[:, 0:n], blk64[:], s2[:, 0:n], True, True, [blk64, s2], [pq])
                        rstd_from_ss(pq, n, r_, 1.0 / 64)
                        STT(b_[:, 0:n], b_[:, 0:n], spc(l, gname, 0, 1), r_[:, 0:n], ALU.mult, ALU.mult, [b_, r_, spt], [b_])
                        TS("pool", st[:, 0:n], b_[:, 0:n], extra, 1.0, ALU.mult, ALU.mult, [b_], [st])
                        DMA("sp", dst[g * 128:(g + 1) * 128, t0:t0 + n], st[:, 0:n], rd=[st])
                    for col0, gname, dst, extra in ((O_Q, "qg", QN, 0.125), (O_K, "kg", KN, 1.0)):
                        if ("Q" if col0 == O_Q else "K") not in want: continue
                        for g in range(4):
                            pa = pz(); zmm(pa, col0 + g * 128, n)
                            b_, s2, r_ = qset(); st = stg()
                            bias_ap = inb(col0 // 128 + g)
                            ACT(b_[:, 0:n], pa[:, 0:n], AF.Identity, [pa, spt], [b_], bias=bias_ap, scale=1.0)
                            ACT(s2[:, 0:n], pa[:, 0:n], AF.Square, [pa, spt], [s2], bias=bias_ap, scale=1.0)
                            pend.append((b_, s2, r_, st, gname, extra, dst, g))
                            if len(pend) > 2: finish(*pend.pop(0))
                    for tb in range(n // 128 if "V" in want else 0):
                        pa = pz()
                        for k in range(8):
                            MM(pa[:, :], h[:, k, tb * 128:(tb + 1) * 128], W[:, k, O_V:O_V + 512], k == 0, k == 7, [Ws[O_V // 512], hres[id(h)][k]], [pa])
                        if pend: finish(*pend.pop(0))
                        vt = vst()
                        TT_("dve", vt[:, :, 0:64], pa[:].rearrange("p (h d) -> p h d", h=8), inbv[:].rearrange("p (h d) -> p h d", h=8), ALU.add, [pa, inbv], [vt])
                        DMA("sp", V[t0 + tb * 128:t0 + (tb + 1) * 128, :], vt[:].rearrange("p h d -> p (h d)"), rd=[vt])
                    while pend: finish(*pend.pop(0))
                    if sq_next is not None: square(sq_next)
                    for wn_, col0, func, dst, ng in (("RX", O_RX, AF.Identity, RX, 4), ("RG", O_RG, AF.Gelu_apprx_tanh, RG, 4), ("G3", O_GCV, AF.Sigmoid, G3, 24)):
                        if wn_ not in want: continue
                        for g in range(ng):
                            pa = pz(); zmm(pa, col0 + g * 128, n)
                            st = stg()
                            ACT(st[:, 0:n], pa[:, 0:n], func, [pa, spt], [st], bias=inb(col0 // 128 + g), scale=1.0)
                            DMA("sp", dst[g * 128:(g + 1) * 128, t0:t0 + n], st[:, 0:n], rd=[st])
                seq_ = list(range(NT)) if l < DEPTH - 1 else [0] + list(range(NOWN + 2, NT)) + [NOWN + 1] + list(range(1, NOWN + 1))
                loadX(seq_[0]); square(seq_[0]); prologue(seq_[0], hh[0]); loadX(seq_[1]); square(seq_[1])
                for q_ in range(NT):
                    if q_ + 1 < NT:
                        prologue(seq_[q_ + 1], hh[(q_ + 1) % 2])
                        if q_ + 2 < NT: loadX(seq_[q_ + 2])
                    main(seq_[q_], hh[q_ % 2], seq_[q_ + 2] if q_ + 2 < NT else None)
                S.barrier()

        def phase2_conv(l):
            with contextlib.ExitStack() as ph:
                dg = sb(ph, "dg", [128, 4, 31, 128], BF16)
                for g in range(4):
                    for k in range(31):
                        TS("dve", dg[:, g, k, :], identb[:], spc(l, "cdw", g * 31 + k, 1), None, ALU.mult, None, [identb, spt], [dg])
                upad = sb(ph, "upad", [128, 4, SEQ + 30], BF16)
                py = [pst(ph, f"py{g}") for g in range(4)]
                pm = pst(ph, "pm"); pv = pst(ph, "pv")
                yf = sb(ph, "yf", [128, 4, 512], F32); yb = sb(ph, "yb", [128, 4, 512], BF16); ysq = sb(ph, "ysq", [128, 4, 512], BF16)
                mean = sb(ph, "mean", [128, 512], F32); var = sb(ph, "var", [128, 512], F32); m2 = sb(ph, "m2", [128, 512], F32)
                tt = RR([sb(ph, f"ctt{i}", [128, 512], F32) for i in range(2)])
                stg = RR([sb(ph, f"cst{i}", [128, 512], BF16) for i in range(4)])
                seqs = [(CTX, SEQ, False), (0, CTX, False)] if l < DEPTH - 1 else [(CTX, HALF, True)]
                for (s0, Ls, rhalo) in seqs:
                    for g in range(4):
                        MS("pool", upad[:, g, 0:15], 0.0, [upad])
                        if rhalo:
                            DMA("sp", upad[:, g, 15:30 + Ls], U[g * 128:(g + 1) * 128, s0:s0 + Ls + 15], wr=[upad])
                        else:
                            MS("pool", upad[:, g, 15 + Ls:30 + Ls], 0.0, [upad])
                            DMA("sp", upad[:, g, 15:15 + Ls], U[g * 128:(g + 1) * 128, s0:s0 + Ls], wr=[upad])
                    for t in range(0, Ls, 512):
                        n = min(512, Ls - t)
                        for g in range(4):
                            for k in range(31):
                                MM(py[g][:, 0:n], dg[:, g, k, :], upad[:, g, t + k:t + k + n], k == 0, k == 30, [dg, upad], [py[g]])
                            ACT(yf[:, g, 0:n], py[g][:, 0:n], AF.Identity, [py[g], spt], [yf], bias=spc(l, "cdb", g), scale=1.0)
                            ACT(ysq[:, g, 0:n], py[g][:, 0:n], AF.Square, [py[g], spt], [ysq], bias=spc(l, "cdb", g), scale=1.0)
                            CP("pool", yb[:, g, 0:n], yf[:, g, 0:n], [yf], [yb])
                        for g in range(4):
                            MM(pm[:, 0:n], onesb[:], yb[:, g, 0:n], g == 0, g == 3, [onesb, yb], [pm])
                        for g in range(4):
                            MM(pv[:, 0:n], onesb[:], ysq[:, g, 0:n], g == 0, g == 3, [onesb, ysq], [pv])
                        ACT(mean[:, 0:n], pm[:, 0:n], AF.Identity, [pm], [mean], scale=1.0 / 512, bias=0.0)
                        TT_("dve", m2[:, 0:n], mean[:, 0:n], mean[:, 0:n], ALU.mult, [mean], [m2])
                        STT(var[:, 0:n], pv[:, 0:n], 1.0 / 512, m2[:, 0:n], ALU.mult, ALU.subtract, [pv, m2], [var])
                        ACT(var[:, 0:n], var[:, 0:n], AF.Ln, [var], [var], bias=EPS, scale=1.0)
                        ACT(var[:, 0:n], var[:, 0:n], AF.Exp, [var], [var], scale=-0.5)
                        for g in range(4):
                            t_ = tt(); st = stg()
                            TT_("dve", t_[:, 0:n], yf[:, g, 0:n], mean[:, 0:n], ALU.subtract, [yf, mean], [t_])
                            TT_("dve", t_[:, 0:n], t_[:, 0:n], var[:, 0:n], ALU.mult, [t_, var], [t_])
                            ACT(st[:, 0:n], t_[:, 0:n], AF.Silu, [t_, spt], [st], scale=spc(l, "clg", g), bias=spc(l, "clb", g))
                            DMA("sp", UC[g * 128:(g + 1) * 128, s0 + t:s0 + t + n], st[:, 0:n], rd=[st])
                S.barrier()

        def rnn_gen(l, ph):
            if True:
                rb = sb(ph, "rb", [128, 2, 2, 4, 128], BF16)
                DMA("pool", rb[:], rnnblk_d[l].rearrange("d w g k j -> k d w g j"), wr=[rb])
                hf = sb(ph, "hf", [128, TT], BF16)
                carry = sb(ph, "carry", [128, 1], F32)
                rdg = sb(ph, "rdg", [128, 8, 4, 128], BF16)
                for dgi_ in range(8):
                    for jj in range(4):
                        TS("dve", rdg[:, dgi_, jj, :], identb[:], spc(l, "rcw", dgi_ * 4 + jj), None, ALU.mult, None, [identb, spt], [rdg])
                pcv = RR([pst(ph, f"pcv{i}") for i in range(2)])
                NS = 2048
                def mk(i):
                    return dict(rxp=sb(ph, f"rxp{i}", [128, NS + 3], BF16), ucf=sb(ph, f"ucf{i}", [128, NS], F32), uc16=sb(ph, f"uc16{i}", [128, NS], BF16),
                                r=sb(ph, f"r{i}", [128, NS], F32), ig=sb(ph, f"ig{i}", [128, NS], F32), m=sb(ph, f"m{i}", [128, NS], F32),
                                bb=sb(ph, f"bb{i}", [128, NS], F32), hs=sb(ph, f"hs{i}", [128, NS], F32), rg=sb(ph, f"rg{i}", [128, NS], BF16),
                                st=sb(ph, f"rst{i}", [128, NS], BF16))
                sets = RR([mk(0), mk(1)])
                ppr = RR([pst(ph, f"ppr{i}") for i in range(3)]); ppi = RR([pst(ph, f"ppi{i}") for i in range(3)])
                NSEG = SEQ // NS
                lat = [(CTX + NS * i, NS) for i in range(NSEG)]
                steps = []
                for g in range(4):
                    for d in range(2):
                        half = (l == DEPTH - 1)
                        segs = [(0, CTX, True, True, not half)] + [(s0, n, i == 0, i == NSEG - 1, (not half) or i < NSEG // 2) for i, (s0, n) in enumerate(lat)]
                        if d == 1:
                            segs = [segs[0]] + segs[:0:-1]
                        elif half:
                            segs = segs[:1 + NSEG // 2]
                        for si, sg_ in enumerate(segs):
                            steps.append((g, d, si == 0) + sg_)
                def stageA(step):
                    g, d, first, s0, n, at_start, at_end, own = step
                    dgi = d * 4 + g
                    B = sets()
                    rxp, ucf, uc16, r, ig, m, rgt = (B[k] for k in ("rxp", "ucf", "uc16", "r", "ig", "m", "rg"))
                    rows = slice(g * 128, (g + 1) * 128)
                    if d == 0:
                        if at_start:
                            MS("pool", rxp[:, 0:3], 0.0, [rxp]); DMA("sp", rxp[:, 3:3 + n], RX[rows, s0:s0 + n], wr=[rxp])
                        else:
                            DMA("sp", rxp[:, 0:3 + n], RX[rows, s0 - 3:s0 + n], wr=[rxp])
                        offs = [0, 1, 2, 3]
                    else:
                        if at_end:
                            MS("pool", rxp[:, n:n + 3], 0.0, [rxp]); DMA("sp", rxp[:, 0:n], RX[rows, s0:s0 + n], wr=[rxp])
                        else:
                            DMA("sp", rxp[:, 0:n + 3], RX[rows, s0:s0 + n + 3], wr=[rxp])
                        offs = [3, 2, 1, 0]
                    if d == 1 and own:
                        DMA("sp", rgt[:, 0:n], RG[rows, s0:s0 + n], wr=[rgt])
                    for t in range(0, n, 512):
                        nn = min(512, n - t)
                        pc = pcv()
                        for jj in range(4):
                            MM(pc[:, 0:nn], rdg[:, dgi, jj, :], rxp[:, t + offs[jj]:t + offs[jj] + nn], jj == 0, jj == 3, [rdg, rxp], [pc])
                        ACT(ucf[:, t:t + nn], pc[:, 0:nn], AF.Identity, [pc, spt], [ucf], bias=spc(l, "rcb", dgi), scale=1.0)
                        CP("pool", uc16[:, t:t + nn], ucf[:, t:t + nn], [ucf], [uc16])
                    for t in range(0, n, 512):
                        nn = min(512, n - t)
                        pr = ppr(); pi = ppi()
                        MM(pr[:, 0:nn], rb[:, d, 0, g, :], uc16[:, t:t + nn], True, True, [rb, uc16], [pr])
                        MM(pi[:, 0:nn], rb[:, d, 1, g, :], uc16[:, t:t + nn], True, True, [rb, uc16], [pi])
                        ACT(r[:, t:t + nn], pr[:, 0:nn], AF.Sigmoid, [pr, spt], [r], bias=spc(l, "rba", dgi), scale=1.0)
                        ACT(ig[:, t:t + nn], pi[:, 0:nn], AF.Sigmoid, [pi, spt], [ig], bias=spc(l, "rbx", dgi), scale=1.0)
                    ACT(m[:, 0:n], r[:, 0:n], AF.Exp, [r, c16], [m], scale=c16[:, l, dgi:dgi + 1])
                    ACT(r[:, 0:n], r[:, 0:n], AF.Exp, [r, c8], [r], scale=c8[:, l, dgi:dgi + 1])
                    ACT(m[:, 0:n], m[:, 0:n], AF.Sqrt, [m], [m], scale=-1.0, bias=1.0)
                    return B
                def stageB(step, B):
                    g, d, first, s0, n, at_start, at_end, own = step
                    ucf, r, ig, m, bb, hs, rgt, st = (B[k] for k in ("ucf", "r", "ig", "m", "bb", "hs", "rg", "st"))
                    rows = slice(g * 128, (g + 1) * 128)
                    TT_("dve", bb[:, 0:n], ig[:, 0:n], ucf[:, 0:n], ALU.mult, [ig, ucf], [bb])
                    TT_("dve", bb[:, 0:n], bb[:, 0:n], m[:, 0:n], ALU.mult, [bb, m], [bb])
                    init = 0.0 if first else carry[:, 0:1]
                    if d == 0:
                        S.op("dve", "tensor_tensor_scan", dict(out=hs[:, 0:n], data0=r[:, 0:n], data1=bb[:, 0:n], initial=init, op0=ALU.mult, op1=ALU.add),
                             R([r, bb, carry]), R([hs]))
                        CP("dve", carry[:, 0:1], hs[:, n - 1:n], [hs], [carry])
                        if own:
                            CP("pool", hf[:, s0:s0 + n], hs[:, 0:n], [hs], [hf])
                    else:
                        S.op("dve", "tensor_tensor_scan", dict(out=hs[:, 0:n][:, ::-1], data0=r[:, 0:n][:, ::-1], data1=bb[:, 0:n][:, ::-1],
                             initial=init, op0=ALU.mult, op1=ALU.add), R([r, bb, carry]), R([hs]))
                        CP("dve", carry[:, 0:1], hs[:, 0:1], [hs], [carry])
                        if own:
                            TT_("pool", hs[:, 0:n], hs[:, 0:n], hf[:, s0:s0 + n], ALU.add, [hs, hf], [hs])
                            TT_("pool", st[:, 0:n], hs[:, 0:n], rgt[:, 0:n], ALU.mult, [hs, rgt], [st])
                            DMA("sp", RN[rows, s0:s0 + n], st[:, 0:n], rd=[st])
                yield len(steps)
                cur = stageA(steps[0])
                for si in range(len(steps)):
                    nxt_ = stageA(steps[si + 1]) if si + 1 < len(steps) else None
                    stageB(steps[si], cur)
                    cur = nxt_
                    yield 1

        def na_gen(l, ph):
            if True:
                def mkin(i):
                    return (sb(ph, f"kn{i}", [128, TT], BF16), [sb(ph, f"qz{i}_{h}", [128, TT], BF16) for h in range(2)],
                            sb(ph, f"vs{i}", [128, TT // 128, 130], BF16), sb(ph, f"nb{i}", [128, 2, ntypes, 128], BF16))
                insets = [mkin(0), mkin(1)]
                for (_, qz_, _, _) in insets:
                    for h in range(2): MS("pool", qz_[h][:], 0.0, [qz_[h]])
                def load_in(hg):
                    kn, qz, vs, nb = insets[hg % 2]
                    rows = slice(hg * 128, (hg + 1) * 128)
                    DMA("sp", kn[:], KN[rows, :], wr=[kn])
                    for h in range(2):
                        DMA("sp", qz[h][h * 64:(h + 1) * 64, :], QN[hg * 128 + h * 64:hg * 128 + (h + 1) * 64, :], wr=[qz[h]])
                    DMA("sp", vs[:], V.rearrange("(c p) f -> p c f", p=128)[:, :, hg * 130:(hg + 1) * 130], wr=[vs])
                    for h in range(2):
                        DMA("pool", nb[:, h], nab_d[l, 2 * hg + h].rearrange("t k q -> k t q"), wr=[nb])
                naT = sb(ph, "naT", [128, TT], BF16)
                psA = RR([pst(ph, f"psA{i}") for i in range(2)]); psB = RR([pst(ph, f"psB{i}") for i in range(2)])
                po = RR([pst(ph, f"po{i}") for i in range(2)])
                ptr = pst(ph, "ptr", (128, 1024), BF16)
                pT = RR([sb(ph, f"pT{i}", [128, 8, 128], BF16) for i in range(3)])
                nat = RR([sb(ph, f"nat{i}", [128, 128], BF16) for i in range(2)])
                rc = RR([sb(ph, f"rc{i}", [128, 2], F32) for i in range(2)])
                yield 4 * 2 * ((64 if l < DEPTH - 1 else HALF // 128) + (2 if l < DEPTH - 1 else 0))
                for hg in range(4):
                    rows = slice(hg * 128, (hg + 1) * 128)
                    kn, qz, vs, nb = insets[hg % 2]
                    if hg == 0: load_in(0)
                    if hg + 1 < 4: load_in(hg + 1)
                    units = [(CTX + 128 * p, [(2 + c, tid) for (c, tid) in naplan[p]] + [(0, None), (1, None)]) for p in range(64 if l < DEPTH - 1 else HALF // 128)]
                    if l < DEPTH - 1:
                        units += [(128 * pc, [(0, None), (1, None)]) for pc in range(2)]
                    items = [(ui, h) for ui in range(len(units)) for h in range(2)]
                    st_ = {}
                    def qk(idx):
                        ui, h = items[idx]; q0, chunks = units[ui]
                        pa = psA(); pb = psB(); pt = pT(); nch = len(chunks)
                        for i, (vc, tid) in enumerate(chunks):
                            bank = pa if i < 4 else pb
                            oap = bank[:, (i % 4) * 128:(i % 4 + 1) * 128]
                            MM(oap, kn[:, vc * 128:(vc + 1) * 128], qz[h][:, q0:q0 + 128], True, True, [kn, qz[h]], [bank])
                        loc = [tid for (_, tid) in chunks if tid is not None]
                        if loc:
                            assert loc == list(range(loc[0], loc[0] + len(loc))) and all(t_ is not None for (_, t_) in chunks[:len(loc)])
                            na4 = min(len(loc), 4)
                            TT_("dve", pa[:, 0:na4 * 128], pa[:, 0:na4 * 128], nb[:, h, loc[0]:loc[0] + na4, :].rearrange("p t q -> p (t q)"), ALU.add, [pa, nb], [pa])
                            if len(loc) > 4:
                                TT_("dve", pb[:, 0:128], pb[:, 0:128], nb[:, h, loc[4], :], ALU.add, [pb, nb], [pb])
                        na_ = min(nch, 4)
                        ACT(pt[:, 0:na_, :], pa[:, 0:na_ * 128].rearrange("p (c q) -> p c q", q=128), AF.Exp, [pa], [pt])
                        if nch > 4:
                            ACT(pt[:, 4:nch, :], pb[:, 0:(nch - 4) * 128].rearrange("p (c q) -> p c q", q=128), AF.Exp, [pb], [pt])
                        st_[idx] = pt
                    def pv(idx):
                        ui, h = items[idx]; q0, chunks = units[ui]; nch = len(chunks)
                        if h == 0:
                            st_["o"] = (po(), nat(), rc())
                        o_ps, nt, rc_ = st_["o"]; pt = st_.pop(idx)
                        for i, (vc, tid) in enumerate(chunks):
                            MM(o_ps[:, h * 65:(h + 1) * 65], pt[:, i, :], vs[:, vc, h * 65:(h + 1) * 65], i == 0, i == nch - 1, [pt, vs], [o_ps])
                        if h == 1:
                            S.op("dve", "reciprocal", dict(out=rc_[:, 0:2], in_=o_ps[:, 64:130:65]), R([o_ps]), R([rc_]))
                            for hh_ in range(2):
                                TS("dve", nt[:, hh_ * 64:(hh_ + 1) * 64], o_ps[:, hh_ * 65:hh_ * 65 + 64], rc_[:, hh_:hh_ + 1], None, ALU.mult, None, [o_ps, rc_], [nt])
                            S.op("pe", "transpose", dict(out=ptr[:, 0:128], in_=nt[:], identity=identb[:]), R([nt, identb]), R([ptr]))
                            ACT(naT[:, q0:q0 + 128], ptr[:, 0:128], AF.Identity, [ptr], [naT])
                    qk(0)
                    for idx in range(len(items)):
                        if idx + 1 < len(items): qk(idx + 1)
                        pv(idx)
                        yield 1
                    DMA("sp", NA[rows, :], naT[:], rd=[naT])

        def phase2_rnn_na(l):
            for gen in (rnn_gen, na_gen):
                with contextlib.ExitStack() as ph:
                    for _ in gen(l, ph): pass
                    S.barrier()

        def phase3a(l, Xsrc):
            with contextlib.ExitStack() as ph:
                pw = [sb(ph, f"pw{i}", [128, 4, D], BF16) for i in range(3)]
                for i, src in enumerate((conv_pw, na_out, rnn_out)):
                    DMA("pool", pw[i][:], src[l].rearrange("(k p) n -> p k n", p=128), wr=[pw[i]])
                ow = sb(ph, "ow", [128, 8, D], BF16)
                for s in range(2):
                    DMA("pool", ow[:, :, s * 512:(s + 1) * 512], out_w[l].rearrange("(k p) n -> p k n", p=128)[:, :, s * 512:(s + 1) * 512], wr=[ow])
                rw = sb(ph, "rw", [128, 8, NE], F32); rbias = sb(ph, "rbias", [128, 4 * NE], F32)
                DMA("sp", rw[:], router_w.rearrange("(k p) e -> p k e", p=128), wr=[rw]); DMA("sp", rbias[:], rbias_d[:, :], wr=[rbias])
                def mk(i):
                    return dict(br=[sb(ph, f"br{i}_{b}", [128, 4, 512], BF16) for b in range(3)], g3=sb(ph, f"g3{i}", [128, 24, 512], BF16),
                                x=sb(ph, f"x{i}", [128, 8, 512], F32))
                sets = RR([mk(0), mk(1)])
                mT = sb(ph, "mT", [128, 8, 512], BF16)
                pbr = RR([pst(ph, f"pb{b}") for b in range(5)])
                py = RR([pst(ph, f"p3y{i}") for i in range(2)])
                pss = pst(ph, "pss3"); prt = pss; ptg = pss
                tsr = RR([[sb(ph, f"t3_{i}_{b}", [128, 512], F32) for b in range(3)] for i in range(2)])
                sq = sb(ph, "sq3", [128, 8, 512], BF16); hn = sb(ph, "hn3", [128, 8, 512], F32); rs = sb(ph, "rs3", [128, 512], F32)
                h2o = sb(ph, "h2o", [128, 8, 512], BF16); gt = sb(ph, "gt3", [NE, 512], F32)
                hns = [S.res(f"hn{k}") for k in range(8)]
                def sm(name, w): return sb(ph, name, [128, w], F32)
                sc = sm("sc", 64); sl = sm("sl", 64); s2 = sm("s2", 64); msk = sm("msk", 64); wt = sm("wt", 64); gate = sm("gate", 64)
                m1 = sm("m1", 16); m2 = sm("m2", 16); gsc = sm("gsc", 16); goh = sm("goh", 16); gm = sm("gm", 4); ws = sm("ws", 4)
                def bc(ap, shape):
                    return ap.unsqueeze(2).broadcast_to(shape)
                mysubs = [s for s in SUBT if not (l == DEPTH - 1 and (s[2] == 1 or s[0] >= CTX + HALF))]
                def front(t0, n, j):
                    B = sets(); X_ = B["x"]
                    for b, src in enumerate((UC, NA, RN)):
                        DMA("sp", B["br"][b][:, :, 0:n], src[:, t0:t0 + n].rearrange("(g p) t -> p g t", p=128), wr=[B["br"][b]])
                    DMA("sp", B["g3"][:, :, 0:n], G3[:, t0:t0 + n].rearrange("(g p) t -> p g t", p=128), wr=[B["g3"]])
                    DMA("sp", X_[:, :, 0:n], Xsrc[:, t0:t0 + n].rearrange("(k p) t -> p k t", p=128), wr=[X_])
                    for og in range(8):
                        ts_ = tsr()
                        for b in range(3):
                            p_ = pbr()
                            for k in range(4):
                                MM(p_[:, 0:n], pw[b][:, k, og * 128:(og + 1) * 128], B["br"][b][:, k, 0:n], k == 0, k == 3, [pw[b], B["br"][b]], [p_])
                            TT_("dve", ts_[b][:, 0:n], p_[:, 0:n], B["g3"][:, b * 8 + og, 0:n], ALU.mult, [p_, B["g3"]], [ts_[b]])
                        TT_("pool", ts_[0][:, 0:n], ts_[0][:, 0:n], ts_[1][:, 0:n], ALU.add, [ts_[0], ts_[1]], [ts_[0]])
                        TT_("pool", mT[:, og, 0:n], ts_[0][:, 0:n], ts_[2][:, 0:n], ALU.add, [ts_[0], ts_[2]], [mT])
                    for og in range(8):
                        p_ = py()
                        for k in range(8):
                            MM(p_[:, 0:n], ow[:, k, og * 128:(og + 1) * 128], mT[:, k, 0:n], k == 0, k == 7, [ow, mT], [p_])
                        STT(X_[:, og, 0:n], p_[:, 0:n], modv[:, l, j, 16 + og:17 + og], X_[:, og, 0:n], ALU.mult, ALU.add, [p_, modv, X_], [X_])
                    DMA("sp", XM[:, t0:t0 + n].rearrange("(k p) t -> p k t", p=128), X_[:, :, 0:n], rd=[X_])
                    return X_
                def tail(t0, n, j, X_):
                    nb = n // 128
                    ACT(sq[:, :, 0:n], X_[:, :, 0:n], AF.Square, [X_], [sq])
                    for k in range(8):
                        MM(pss[:, 0:n], onesb[:], sq[:, k, 0:n], k == 0, k == 7, [onesb, sq], [pss])
                    rstd_from_ss(pss, n, rs, 1.0 / D)
                    for k in range(8):
                        TT_("dve", hn[:, k, 0:n], X_[:, k, 0:n], rs[:, 0:n], ALU.mult, [X_, rs], [hns[k]])
                        ACT(hn[:, k, 0:n], hn[:, k, 0:n], AF.Identity, [hns[k], gs2, modv], [hns[k]], scale=gs2[:, l, j, k:k + 1], bias=modv[:, l, j, 24 + k:25 + k])
                        CP("pool", h2o[:, k, 0:n], hn[:, k, 0:n], [hns[k]], [h2o])
                    DMA("sp", H2[:, t0:t0 + n].rearrange("(k p) t -> p k t", p=128), h2o[:, :, 0:n], rd=[h2o])
                    for tb in range(nb):
                        for k in range(8):
                            MM(prt[:, tb * NE:(tb + 1) * NE], hn[:, k, tb * 128:(tb + 1) * 128], rw[:, k, :], k == 0, k == 7, [hns[k], rw], [prt])
                    w = nb * NE; g4 = nb * 4
                    def v3(t_, inner): return t_[:, 0:w].rearrange("p (a e) -> p a e", e=inner)
                    ACT(sc[:, 0:w], prt[:, 0:w], AF.Sigmoid, [prt], [sc])
                    TT_("dve", sl[:, 0:w], sc[:, 0:w], rbias[:, 0:w], ALU.add, [sc, rbias], [sl])
                    S.op("dve", "tensor_reduce", dict(out=m1[:, 0:g4], in_=v3(sl, 4), axis=AX.X, op=ALU.max), R([sl]), R([m1]))
                    TT_("dve", v3(s2, 4), v3(sl, 4), bc(m1[:, 0:g4], [128, g4, 4]), ALU.is_equal, [sl, m1], [s2])
                    STT(s2[:, 0:w], s2[:, 0:w], -1e9, sl[:, 0:w], ALU.mult, ALU.add, [s2, sl], [s2])
                    S.op("dve", "tensor_reduce", dict(out=m2[:, 0:g4], in_=v3(s2, 4), axis=AX.X, op=ALU.max), R([s2]), R([m2]))
                    TT_("dve", gsc[:, 0:g4], m1[:, 0:g4], m2[:, 0:g4], ALU.add, [m1, m2], [gsc])
                    S.op("dve", "tensor_reduce", dict(out=gm[:, 0:nb], in_=gsc[:, 0:g4].rearrange("p (a e) -> p a e", e=4), axis=AX.X, op=ALU.max), R([gsc]), R([gm]))
                    TT_("dve", goh[:, 0:g4].rearrange("p (a e) -> p a e", e=4), gsc[:, 0:g4].rearrange("p (a e) -> p a e", e=4), bc(gm[:, 0:nb], [128, nb, 4]), ALU.is_equal, [gsc, gm], [goh])
                    TT_("dve", v3(msk, 4), v3(sl, 4), bc(m2[:, 0:g4], [128, g4, 4]), ALU.is_ge, [sl, m2], [msk])
                    TT_("dve", v3(msk, 4), v3(msk, 4), bc(goh[:, 0:g4], [128, g4, 4]), ALU.mult, [msk, goh], [msk])
                    TT_("dve", wt[:, 0:w], sc[:, 0:w], msk[:, 0:w], ALU.mult, [sc, msk], [wt])
                    S.op("dve", "tensor_reduce", dict(out=ws[:, 0:nb], in_=v3(wt, NE), axis=AX.X, op=ALU.add), R([wt]), R([ws]))
                    S.op("dve", "reciprocal", dict(out=ws[:, 0:nb], in_=ws[:, 0:nb]), R([ws]), R([ws]))
                    TT_("dve", v3(gate, NE), v3(wt, NE), bc(ws[:, 0:nb], [128, nb, NE]), ALU.mult, [wt, ws], [gate])
                    for tb in range(nb):
                        S.op("pe", "transpose", dict(out=ptg[0:NE, tb * 128:(tb + 1) * 128], in_=gate[:, tb * NE:(tb + 1) * NE], identity=ident[:]), R([gate, ident]), R([ptg]))
                    CP("dve", gt[:, 0:n], ptg[0:NE, 0:n], [ptg], [gt])
                    DMA("sp", GT[:, t0:t0 + n], gt[:, 0:n], rd=[gt])
                xs_ = front(*mysubs[0])
                for i_, s_ in enumerate(mysubs):
                    nx_ = front(*mysubs[i_ + 1]) if i_ + 1 < len(mysubs) else None
                    tail(*s_, xs_)
                    xs_ = nx_
                S.barrier()

        def phase3b(l, final):
            with contextlib.ExitStack() as ph:
                SMAX = 1280
                xm = sb(ph, "xm", [128, 8, SMAX], F32); xms = [S.res(f"xm{k}") for k in range(8)]
                hg = RR([(sb(ph, f"h2b{i}", [128, 8, SMAX], BF16), sb(ph, f"gT{i}", [NE, SMAX], F32)) for i in range(2)])
                selt = sb(ph, "selt", [NE, NE * 128], F32)
                DMA("sp", selt[:], sel_d[:, :], wr=[selt])
                wset = RR([(sb(ph, f"w1b{i}", [128, 8, DE], BF16), sb(ph, f"w3b{i}", [128, 8, DE], BF16), sb(ph, f"w2b{i}", [128, 4, D], BF16)) for i in range(2)])
                pg = RR([pst(ph, f"pg{i}") for i in range(2)])
                p1 = RR([pst(ph, f"p1_{i}") for i in range(2)]); p3 = RR([pst(ph, f"p3_{i}") for i in range(2)]); po = RR([pst(ph, f"po2{i}") for i in range(2)])
                gbs = RR([sb(ph, f"gb{i}", [128, 512], F32) for i in range(4)])
                s1 = RR([sb(ph, f"s1_{i}", [128, 512], F32) for i in range(2)]); tt = RR([sb(ph, f"tt_{i}", [128, 512], F32) for i in range(2)])
                he = RR([sb(ph, f"he{i}", [128, 4, 512], BF16) for i in range(2)])
                subs = [s for s in SUBT if not (l == DEPTH - 1 and (s[2] == 1 or s[0] >= CTX + HALF))]
                supers = [subs[0:3]] + [subs[i:i + 2] for i in range(3, len(subs), 2)] if l < DEPTH - 1 else [subs[i:i + 2] for i in range(0, len(subs), 2)]
                def load_hg(sup):
                    c0 = sup[0][0]; Sn = sum(s[1] for s in sup); h2b, gT = hg()
                    DMA("sp", h2b[:, :, 0:Sn], H2[:, c0:c0 + Sn].rearrange("(k p) t -> p k t", p=128), wr=[h2b])
                    DMA("sp", gT[:, 0:Sn], GT[:, c0:c0 + Sn], wr=[gT])
                    return h2b, gT
                nxt_hg = load_hg(supers[0])
                wcache = {}
                def weights(key, e):
                    if key not in wcache:
                        w1b, w3b, w2b = wset()
                        for half in range(2):
                            DMA("pool", w1b[:, half * 4:(half + 1) * 4, :], w1_d[l, e].rearrange("(k p) n -> p k n", p=128)[:, half * 4:(half + 1) * 4, :], wr=[w1b])
                            DMA("pool", w3b[:, half * 4:(half + 1) * 4, :], w3_d[l, e].rearrange("(k p) n -> p k n", p=128)[:, half * 4:(half + 1) * 4, :], wr=[w3b])
                            DMA("pool", w2b[:, half * 2:(half + 1) * 2, :], w2_d[l, e].rearrange("(k p) n -> p k n", p=128)[:, half * 2:(half + 1) * 2, :], wr=[w2b])
                        wcache[key] = (w1b, w3b, w2b)
                    return wcache[key]
                for si, sup in enumerate(supers):
                    c0 = sup[0][0]; Sn = sum(s[1] for s in sup)
                    h2b, gT = nxt_hg
                    for og in range(8):
                        S.dma("sp", xm[:, og, 0:Sn], XM[og * 128:(og + 1) * 128, c0:c0 + Sn], [], [xms[og]])
                    ulist = [(e, s_) for e in range(NE) for s_ in sup]
                    def gate_bc(ui):
                        e, (t0, n, j) = ulist[ui]; g_ = gbs()
                        DMA("sp", g_[:, 0:n], GT[e:e + 1, t0:t0 + n].broadcast_to([128, n]), wr=[g_])
                        return g_
                    def down(ui, he_):
                        e, (t0, n, j) = ulist[ui]; o = t0 - c0; w2b = weights((si, e), e)[2]
                        for og in range(8):
                            p_ = po()
                            for k in range(4):
                                MM(p_[:, 0:n], w2b[:, k, og * 128:(og + 1) * 128], he_[:, k, 0:n], k == 0, k == 3, [w2b, he_], [p_])
                            S.op("dve", "scalar_tensor_tensor", dict(out=xm[:, og, o:o + n], in0=p_[:, 0:n], scalar=modv[:, l, j, 40 + og:41 + og], in1=xm[:, og, o:o + n],
                                 op0=ALU.mult, op1=ALU.add), [p_.res, modv.res, xms[og]], [xms[og]])
                    gq = [gate_bc(0)] + ([gate_bc(1)] if len(ulist) > 1 else []); prev = None
                    for ui, (e, (t0, n, j)) in enumerate(ulist):
                        o = t0 - c0; w1b, w3b, w2b = weights((si, e), e)
                        he_ = he()
                        for fc in range(4):
                            p1_ = p1(); p3_ = p3()
                            for k in range(8):
                                MM(p1_[:, 0:n], w1b[:, k, fc * 128:(fc + 1) * 128], h2b[:, k, o:o + n], k == 0, k == 7, [w1b, h2b], [p1_])
                            for k in range(8):
                                MM(p3_[:, 0:n], w3b[:, k, fc * 128:(fc + 1) * 128], h2b[:, k, o:o + n], k == 0, k == 7, [w3b, h2b], [p3_])
                            s1_ = s1(); t_ = tt()
                            ACT(s1_[:, 0:n], p1_[:, 0:n], AF.Silu, [p1_], [s1_])
                            TT_("dve", t_[:, 0:n], s1_[:, 0:n], p3_[:, 0:n], ALU.mult, [s1_, p3_], [t_])
                            TT_("pool", he_[:, fc, 0:n], t_[:, 0:n], gq[0][:, 0:n], ALU.mult, [t_, gq[0]], [he_])
                        gq.pop(0)
                        if ui + 2 < len(ulist):
                            gq.append(gate_bc(ui + 2))
                        if prev is not None: down(*prev)
                        prev = (ui, he_)
                        if (ui == 0 or ulist[ui - 1][0] != e):
                            if e + 1 < NE:
                                weights((si, e + 1), e + 1)
                            elif si + 1 < len(supers):
                                weights((si + 1, 0), 0)
                        if ui == 0 and si + 1 < len(supers):
                            nxt_hg = load_hg(supers[si + 1])
                    down(*prev)
                    dst = outT[:, c0 - CTX:c0 - CTX + Sn] if final else XN[:, c0:c0 + Sn]
                    for og in range(8):
                        S.dma("sp", dst[og * 128:(og + 1) * 128, :], xm[:, og, 0:Sn], [xms[og]], [])
                S.barrier()

        order = ["p1", "conv", "rnn", "na", "p3a", "p3b"]
        def run_layer(l, Xsrc, stop=None):
            for name, fn in (("p1", lambda: (phase1(l, Xsrc, preW0 if l == 0 else None), w0stack.close() if l == 0 else None)), ("conv", lambda: phase2_conv(l)), ("na", lambda: phase2_rnn_na(l)), ("p3a", lambda: phase3a(l, Xsrc)), ("p3b", lambda: phase3b(l, l == DEPTH - 1))):
                fn()
                if stop == name:
                    return True
            return False
        if upto == "all":
            run_layer(0, xT); run_layer(1, XN)
        elif upto.startswith("only:"):
            {"conv": lambda: phase2_conv(0), "na": lambda: phase2_rnn_na(0)}[upto[5:]]()
        elif upto == "l0":
            run_layer(0, xT)
        else:
            run_layer(0, xT, stop=upto)
        for n_, ap in dump_out.items():
            S.dma("sp", ap, scr[n_])
        S.barrier()
        S.emit()
    return nc


def prep_shared(inp, rev=False):
    types, _ = na_geometry()
    sh = {}
    dsl = slice(None, None, -1) if rev else slice(None)
    sh["ident_in"] = np.eye(128, dtype=np.float32)
    sh["sp"] = np.stack([pack_small(inp, l, rev) for l in range(DEPTH)])
    sh["inbv"] = np.stack([np.ascontiguousarray(np.broadcast_to(inp["in_b"][l, O_V:O_V + 512], (128, 512))) for l in range(DEPTH)])
    sh["rbias"] = np.ascontiguousarray(np.broadcast_to(np.tile(inp["router_bias"], 4), (128, 4 * NE)))
    for k in ("mod_w", "in_w", "conv_pw_w", "na_out_w", "rnn_out_w", "out_w", "exp_w1", "exp_w3", "exp_w2", "router_w"):
        sh[k] = np.ascontiguousarray(inp[k], dtype=np.float32)
    blk = np.zeros((DEPTH, 2, 2, 4, 128, 128), np.float32)
    for l in range(DEPTH):
        for d in range(2):
            for wi, wn in enumerate(("rnn_wa", "rnn_wx")):
                w = inp[wn][l][dsl][d]
                for n in range(8):
                    g, o = n // 2, (n % 2) * 64
                    blk[l, d, wi, g, o:o + 64, o:o + 64] = w[n]
    sh["rnnblk"] = blk
    sh["nabias"] = np.stack([build_na_bias(inp["na_rpb"][l], rev) for l in range(DEPTH)])
    sel = np.zeros((NE, NE * 128), np.float32)
    for e in range(NE):
        sel[e, e * 128:(e + 1) * 128] = 1.0
    sh["sel"] = sel
    return sh


def prep_core(inp, b, rev=False):
    m = {}
    dsl = slice(None, None, -1) if rev else slice(None)
    m["xT"] = np.ascontiguousarray(np.concatenate([inp["ctx"][b][dsl].T, inp["x"][b][dsl].T], axis=1), dtype=np.float32)
    cp = np.stack([_fm(inp["c"][b]), _fm(inp["c_ctx"])], axis=-1)
    m["c_pk"] = np.ascontiguousarray(cp.reshape(128, 16), dtype=np.float32)
    return m


_NC_CACHE = {}


def kernel(**inputs):
    inp = {k: np.asarray(v) for k, v in inputs.items()}
    if "full" not in _NC_CACHE:
        _NC_CACHE["full"] = build()
    nc = _NC_CACHE["full"]
    shs = [prep_shared(inp, False), prep_shared(inp, True)]
    in_maps = []
    for core in range(8):
        rev = core >= 4
        m = dict(shs[int(rev)]); m.update(prep_core(inp, core % 4, rev)); in_maps.append(m)
    res = run_bass_kernel_spmd(nc, in_maps, core_ids=list(range(8)))
    out = np.empty((4, SEQ, D), np.float32)
    for b in range(4):
        out[b, :HALF] = res.results[b]["outT"].T
        out[b, HALF:] = res.results[b + 4]["outT"].T[::-1]
    return out
```

```python
import contextlib
import numpy as np
import concourse.bass as bass
import concourse.mybir as mybir
from concourse.bass_utils import run_bass_kernel_spmd

F32 = mybir.dt.float32
BF16 = mybir.dt.bfloat16
AF = mybir.ActivationFunctionType
ALU = mybir.AluOpType
AX = mybir.AxisListType

D = 1024; SEQ = 8192; CTX = 256; TT = SEQ + CTX; DEPTH = 2
DIN = 6656; NE = 16; DE = 512
EPS = 1e-6
NEG = -30000.0
O_A, O_GA, O_Q, O_K, O_V, O_RX, O_RG, O_GCV, O_GNA, O_GRN = 0, 512, 1024, 1536, 2048, 2560, 3072, 3584, 4608, 5632

SP_FIELDS = [("mod_b", 48), ("n1g", 8), ("n2g", 8), ("in_b", 52), ("cdw", 124), ("cdb", 4), ("clg", 4), ("clb", 4),
             ("qg", 1), ("kg", 1), ("rcw", 32), ("rcb", 8), ("rba", 8), ("rbx", 8), ("lam", 8)]
SP_OFF = {}
_o = 0
for _n, _w in SP_FIELDS:
    SP_OFF[_n] = (_o, _w); _o += _w
SP_W = _o


def _fm(v):
    return np.ascontiguousarray(v.reshape(-1, 128).T)


def pack_small(inp, l, rev=False):
    sp = np.zeros((128, SP_W), np.float32)
    dsl = slice(None, None, -1) if rev else slice(None)
    def put(name, arr):
        o, w = SP_OFF[name]; assert arr.shape == (128, w), (name, arr.shape); sp[:, o:o + w] = arr
    put("mod_b", _fm(inp["mod_b"][l])); put("n1g", _fm(inp["norm1_g"][l])); put("n2g", _fm(inp["norm2_g"][l]))
    put("in_b", _fm(inp["in_b"][l]))
    cw = inp["conv_dw_w"][l][dsl]
    put("cdw", np.ascontiguousarray(cw.T.reshape(4, 128, 31).transpose(1, 0, 2)).reshape(128, 124))
    put("cdb", _fm(inp["conv_dw_b"][l])); put("clg", _fm(inp["conv_ln_g"][l])); put("clb", _fm(inp["conv_ln_b"][l]))
    put("qg", np.tile(inp["na_q_g"][l], 2)[:, None]); put("kg", np.tile(inp["na_k_g"][l], 2)[:, None])
    rw = inp["rnn_conv_w"][l][dsl]
    put("rcw", np.ascontiguousarray(rw.transpose(2, 0, 1).reshape(4, 128, 2, 4).transpose(1, 2, 0, 3)).reshape(128, 32))
    def dg(a):
        return np.ascontiguousarray(a.reshape(2, 4, 128).transpose(2, 0, 1)).reshape(128, 8)
    put("rcb", dg(inp["rnn_conv_b"][l][dsl])); put("rba", dg(inp["rnn_ba"][l][dsl])); put("rbx", dg(inp["rnn_bx"][l][dsl]))
    put("lam", dg(inp["rnn_lam"][l][dsl]))
    return sp


def na_geometry():
    types = {}; plan = []
    for p in range(64):
        rs0 = min(max(2 * p - 4, 0), 120); rs1 = min(max(2 * p + 1 - 4, 0), 120)
        lo = min(rs0, rs1); hi = max(rs0, rs1) + 7
        lst = []
        for c in range(lo // 2, hi // 2 + 1):
            key = (c - p, rs0 - 2 * p, rs1 - (2 * p + 1))
            if key not in types:
                types[key] = len(types)
            lst.append((c, types[key]))
        plan.append(lst)
    return types, plan


def na_true_tile(rpb, p, c):
    k = np.arange(128)[:, None]; q = np.arange(128)[None, :]
    krow = 2 * c + k // 64; kc = k % 64; qrow = 2 * p + q // 64; qc = q % 64
    rs = np.clip(qrow - 4, 0, 120); cs = np.clip(qc - 8, 0, 48)
    valid = (krow >= rs) & (krow < rs + 8) & (kc >= cs) & (kc < cs + 16)
    dr = np.clip(krow - qrow + 7, 0, 14); dc = np.clip(kc - qc + 15, 0, 30)
    val = rpb[:, dr, dc]
    return np.where(valid[None], val, np.float32(NEG)).astype(np.float32)


def build_na_bias(rpb, rev=False):
    types, plan = na_geometry()
    H = rpb.shape[0]
    out = np.full((H, len(types), 128, 128), NEG, np.float32)
    done = set()
    for p in range(64):
        for (c, tid) in plan[p]:
            if tid in done: continue
            done.add(tid)
            out[:, tid] = na_true_tile(rpb, 63 - p, 63 - c)[:, ::-1, ::-1] if rev else na_true_tile(rpb, p, c)
    return out


class Res:
    __slots__ = ("name", "w", "r")
    def __init__(self, name):
        self.name = name; self.w = None; self.r = {}


class Tl:
    __slots__ = ("t", "res")
    def __init__(self, t, res):
        self.t = t; self.res = res
    def __getitem__(self, idx):
        return self.t[idx]


class Sched:
    SEM_LIMIT = 30000
    def __init__(self, nc, stack, n_sems=84, dma_pool=10):
        self.nc = nc
        self.engs = {"pe": nc.tensor, "dve": nc.vector, "act": nc.scalar, "pool": nc.gpsimd, "sp": nc.sync}
        self.free_sems = [stack.enter_context(nc.semaphore(f"s{i}")) for i in range(n_sems)]
        self.q = {k: [] for k in self.engs}
        self.sem = {k: self.free_sems.pop() for k in self.engs}
        self.cnt = {k: 0 for k in self.engs}
        self.waited = {k: {} for k in self.engs}
        self.pend = {k: ([], []) for k in self.engs}
        self.dma_sems = {k: [[self.free_sems.pop(), 0] for _ in range(dma_pool)] for k in ("sp", "pool", "act")}
        self.dma_rr = {k: 0 for k in self.dma_sems}
        self.last_ev = {}
        self.nres = 0

    def res(self, name=None):
        self.nres += 1
        return Res(name or f"r{self.nres}")

    def _wait(self, eng, ev):
        sem, val = ev
        key = id(sem)
        if self.waited[eng].get(key, 0) >= val:
            return
        self.waited[eng][key] = val
        self.q[eng].append(lambda e, sem=sem, val=val: e.wait_ge(sem, val))

    def _deps(self, eng, reads, writes, skip_same):
        deps = {}
        def add(ev):
            if ev is None: return
            k = id(ev[0])
            if k not in deps or deps[k][1] < ev[1]: deps[k] = ev
        for r in reads: add(r.w)
        for w in writes:
            add(w.w)
            for ev in w.r.values(): add(ev)
        own = id(self.sem[eng])
        for k, ev in deps.items():
            if skip_same and k == own: continue
            self._wait(eng, ev)

    def _commit(self, ev, reads, writes):
        for w in writes:
            w.w = ev; w.r = {}
        for r in reads:
            k = id(ev[0])
            if k not in r.r or r.r[k][1] < ev[1]: r.r[k] = ev

    def op(self, eng, meth, kw, reads=(), writes=(), sig=True):
        self._deps(eng, reads, writes, skip_same=(eng == "pe"))
        fn = lambda e, meth=meth, kw=kw: getattr(e, meth)(**kw)
        if not sig:
            self.pend[eng][0].extend(reads); self.pend[eng][1].extend(writes)
            self.q[eng].append(lambda e, fn=fn: fn(e))
            return None
        if self.cnt[eng] >= self.SEM_LIMIT:
            self.sem[eng] = self.free_sems.pop(); self.cnt[eng] = 0
        self.cnt[eng] += 1
        sem = self.sem[eng]; ev = (sem, self.cnt[eng])
        self.q[eng].append(lambda e, fn=fn, sem=sem: fn(e).then_inc(sem, 1))
        pr, pw = self.pend[eng]
        self._commit(ev, list(reads) + pr, list(writes) + pw)
        self.pend[eng] = ([], [])
        self.last_ev[eng] = ev
        return ev

    def dma(self, qn, out, in_, reads=(), writes=()):
        self._deps(qn, reads, writes, skip_same=False)
        slot = self.dma_sems[qn][self.dma_rr[qn] % len(self.dma_sems[qn])]
        self.dma_rr[qn] += 1
        if slot[1] >= self.SEM_LIMIT:
            slot[0] = self.free_sems.pop(); slot[1] = 0
        if slot[1] > 0:
            self._wait(qn, (slot[0], slot[1]))
        slot[1] += 16
        sem = slot[0]; ev = (sem, slot[1])
        self.q[qn].append(lambda e, out=out, in_=in_, sem=sem: e.dma_start(out=out, in_=in_).then_inc(sem, 16))
        self._commit(ev, reads, writes)
        return ev

    def barrier(self):
        evs = [ev for ev in self.last_ev.values()]
        for qn, slots in self.dma_sems.items():
            for s in slots:
                if s[1] > 0: evs.append((s[0], s[1]))
        for eng in self.engs:
            for ev in evs:
                if id(ev[0]) == id(self.sem[eng]) and eng == "pe": continue
                self._wait(eng, ev)

    def emit(self):
        with self.nc.Block() as block:
            for name, attr in (("pe", "tensor"), ("dve", "vector"), ("act", "scalar"), ("pool", "gpsimd"), ("sp", "sync")):
                q = self.q[name]
                def body(e, q=q):
                    for f in q: f(e)
                getattr(block, attr)(body)


SUBT = [(0, CTX, 1)] + [(CTX + 512 * i, 512, 0) for i in range(16)]
HALF = SEQ // 2; NOWN = HALF // 512
VW = 8 * 65


def build(upto="all", dump=()):
    nc = bass.Bass("TRN2", target_bir_lowering=False)
    def din(name, shape, dt=F32):
        return nc.dram_tensor(name, list(shape), dt, kind="ExternalInput").ap()
    def dscr(name, shape, dt):
        return nc.dram_tensor(name, list(shape), dt, kind="Internal").ap()
    xT = din("xT", [D, TT]); c_pk = din("c_pk", [128, 16]); ident_d = din("ident_in", [128, 128])
    sp_d = din("sp", [DEPTH, 128, SP_W]); inbv_d = din("inbv", [DEPTH, 128, 512]); rbias_d = din("rbias", [128, 4 * NE])
    mod_w = din("mod_w", [DEPTH, D, 6 * D]); in_w = din("in_w", [DEPTH, D, DIN])
    conv_pw = din("conv_pw_w", [DEPTH, 512, D]); na_out = din("na_out_w", [DEPTH, 512, D])
    rnn_out = din("rnn_out_w", [DEPTH, 512, D]); out_w = din("out_w", [DEPTH, D, D])
    w1_d = din("exp_w1", [DEPTH, NE, D, DE]); w3_d = din("exp_w3", [DEPTH, NE, D, DE]); w2_d = din("exp_w2", [DEPTH, NE, DE, D])
    router_w = din("router_w", [D, NE]); rnnblk_d = din("rnnblk", [DEPTH, 2, 2, 4, 128, 128])
    natypes, naplan = na_geometry(); ntypes = len(natypes)
    nab_d = din("nabias", [DEPTH, 8, ntypes, 128, 128]); sel_d = din("sel", [NE, NE * 128])
    outT = nc.dram_tensor("outT", [D, HALF], F32, kind="ExternalOutput").ap()
    U = dscr("U", [512, TT], BF16); QN = dscr("QN", [512, TT], BF16); KN = dscr("KN", [512, TT], BF16)
    V = dscr("V", [TT, VW], BF16); RX = dscr("RX", [512, TT], BF16); RG = dscr("RG", [512, TT], BF16)
    G3 = dscr("G3", [3 * D, TT], BF16); UC = dscr("UC", [512, TT], BF16); NA = dscr("NA", [512, TT], BF16)
    RN = dscr("RN", [512, TT], BF16); XM = dscr("XM", [D, TT], F32); XN = dscr("XN", [D, TT], F32)
    H2 = dscr("H2", [D, TT], BF16); GT = dscr("GT", [NE, TT], F32)
    scr = dict(H2=H2, GT=GT, U=U, QN=QN, KN=KN, V=V, RX=RX, RG=RG, G3=G3, UC=UC, NA=NA, RN=RN, XM=XM, XN=XN)
    dump_out = {n: nc.dram_tensor("dump_" + n, list(scr[n].shape), scr[n].dtype, kind="ExternalOutput").ap() for n in dump}

    with contextlib.ExitStack() as top:
        S = Sched(nc, top)
        uid = [0]
        def sb(stack, name, shape, dt):
            uid[0] += 1
            return Tl(stack.enter_context(nc.sbuf_tensor(f"{name}_{uid[0]}", list(shape), dt)), S.res(name))
        def pst(stack, name, shape=(128, 512), dt=F32):
            uid[0] += 1
            return Tl(stack.enter_context(nc.psum_tensor(f"{name}_{uid[0]}", list(shape), dt)), S.res(name))
        def R(ts): return [t.res if hasattr(t, 'res') else t for t in ts]
        def ACT(out, in_, func, rd, wr, **kw):
            S.op("act", "activation", dict(out=out, in_=in_, func=func, **kw), R(rd), R(wr))
        def MM(out, lhsT, rhs, start, stop, rd, wr):
            S.op("pe", "matmul", dict(out=out, lhsT=lhsT, rhs=rhs, start=start, stop=stop), R(rd), R(wr), sig=stop)
        def TT_(eng, out, in0, in1, op, rd, wr):
            S.op(eng, "tensor_tensor", dict(out=out, in0=in0, in1=in1, op=op), R(rd), R(wr))
        def STT(out, in0, scalar, in1, op0, op1, rd, wr):
            S.op("dve", "scalar_tensor_tensor", dict(out=out, in0=in0, scalar=scalar, in1=in1, op0=op0, op1=op1), R(rd), R(wr))
        def TS(eng, out, in0, s1, s2, op0, op1, rd, wr):
            kw = dict(out=out, in0=in0, scalar1=s1, scalar2=s2, op0=op0)
            if op1 is not None: kw["op1"] = op1
            S.op(eng, "tensor_scalar", kw, R(rd), R(wr))
        def CP(eng, out, in_, rd, wr):
            S.op(eng, "tensor_copy", dict(out=out, in_=in_), R(rd), R(wr))
        def MS(eng, ap, val, wr):
            S.op(eng, "memset", dict(ap=ap, constant=val), [], R(wr))
        def DMA(q, out, in_, rd=(), wr=()):
            S.dma(q, out, in_, R(rd), R(wr))
        class RR:
            def __init__(self, lst): self.l = lst; self.i = -1
            def __call__(self):
                self.i += 1; return self.l[self.i % len(self.l)]

        ident = sb(top, "ident", [128, 128], F32); identb = sb(top, "identb", [128, 128], BF16)
        onesb = sb(top, "onesb", [128, 128], BF16)
        blk64 = sb(top, "blk64", [128, 128], BF16)
        spt = sb(top, "spt", [128, DEPTH, SP_W], F32)
        cact = sb(top, "cact", [128, 8, 2], F32)
        modv = sb(top, "modv", [128, DEPTH, 2, 48], F32)
        gs1 = sb(top, "gs1", [128, DEPTH, 2, 8], F32); gs2 = sb(top, "gs2", [128, DEPTH, 2, 8], F32)
        c8 = sb(top, "c8", [128, DEPTH, 8], F32); c16 = sb(top, "c16", [128, DEPTH, 8], F32)
        def spc(l, name, i=0, w=1):
            o, _ = SP_OFF[name]; return spt[:, l, o + i:o + i + w]

        def load_W(l, stack):
            W = sb(stack, "W", [128, 8, DIN], BF16)
            Ws = [S.res(f"W{s}") for s in range(13)]
            order = list(range(13)) if l < DEPTH - 1 else [5, 3, 4, 0, 1, 2, 6, 7, 8, 9, 10, 11, 12]
            for s in order:
                DMA("pool", W[:, :, s * 512:(s + 1) * 512], in_w[l].rearrange("(k p) n -> p k n", p=128)[:, :, s * 512:(s + 1) * 512], wr=[Ws[s]])
            return W, Ws

        w0stack = top.enter_context(contextlib.ExitStack())
        preW0 = load_W(0, w0stack)
        with contextlib.ExitStack() as ph:
            ps0 = pst(ph, "ps0")
            mwb = RR([sb(ph, f"mwb{i}", [128, 8, 512], F32) for i in range(2)])
            ctmp = sb(ph, "ctmp", [128, 16], F32)
            DMA("sp", ident[:], ident_d[:, :], wr=[ident])
            DMA("sp", spt[:], sp_d.rearrange("l p w -> p l w"), wr=[spt])
            DMA("sp", ctmp[:], c_pk[:, :], wr=[ctmp])
            CP("dve", identb[:], ident[:], [ident], [identb])
            MS("dve", onesb[:], 1.0, [onesb]); MS("dve", blk64[:], 0.0, [blk64])
            MS("dve", blk64[0:64, 0:64], 1.0, [blk64]); MS("dve", blk64[64:128, 64:128], 1.0, [blk64])
            ACT(cact[:].rearrange("p k j -> p (k j)"), ctmp[:], AF.Silu, [ctmp], [cact])
            for l in range(DEPTH):
                ACT(c8[:, l, :], spc(l, "lam", 0, 8), AF.Exp, [spt], [c8], scale=-1.0)
                ACT(c8[:, l, :], c8[:, l, :], AF.Ln, [c8], [c8], bias=1.0, scale=1.0)
                TS("dve", c16[:, l, :], c8[:, l, :], -16.0, None, ALU.mult, None, [c8], [c16])
                TS("dve", c8[:, l, :], c8[:, l, :], -8.0, None, ALU.mult, None, [c8, c16], [c8])
            for l in range(DEPTH):
                for s in range(12):
                    buf = mwb()
                    DMA("sp", buf[:], mod_w[l].rearrange("(k p) n -> p k n", p=128)[:, :, s * 512:(s + 1) * 512], wr=[buf])
                    for g in range(4):
                        gg = s * 4 + g
                        for k in range(8):
                            MM(ps0[:, 2 * gg:2 * gg + 2], buf[:, k, g * 128:(g + 1) * 128], cact[:, k, :], k == 0, k == 7, [buf, cact], [ps0])
                for j in range(2):
                    TT_("dve", modv[:, l, j, :], ps0[:, j:96:2], spc(l, "mod_b", 0, 48), ALU.add, [ps0, spt], [modv])
                    STT(gs1[:, l, j, :], modv[:, l, j, 8:16], 1.0, spc(l, "n1g", 0, 8), ALU.add, ALU.mult, [modv, spt], [gs1])
                    STT(gs2[:, l, j, :], modv[:, l, j, 32:40], 1.0, spc(l, "n2g", 0, 8), ALU.add, ALU.mult, [modv, spt], [gs2])
            S.barrier()

        def rstd_from_ss(ss_ps, n, rs, inv_n):
            ACT(rs[:, 0:n], ss_ps[:, 0:n], AF.Ln, [ss_ps], [rs], scale=inv_n, bias=EPS)
            ACT(rs[:, 0:n], rs[:, 0:n], AF.Exp, [rs], [rs], scale=-0.5)

        def phase1(l, Xsrc, pre=None):
            with contextlib.ExitStack() as ph:
                W, Ws = pre if pre is not None else load_W(l, ph)
                inbv = sb(ph, "inbv", [128, 512], F32)
                DMA("sp", inbv[:], inbv_d[l], wr=[inbv])
                X = sb(ph, "X", [128, 8, 512], F32); sq = sb(ph, "sq", [128, 8, 512], BF16)
                rs = sb(ph, "rs", [128, 512], F32); hh = [sb(ph, f"h{i}", [128, 8, 512], BF16) for i in range(2)]
                hres = {id(hh[b_]): [S.res(f"h{b_}_{k}") for k in range(8)] for b_ in range(2)}
                pss = pst(ph, "pss"); pq = pst(ph, "pq")
                pz = RR([pst(ph, f"pz{i}") for i in range(5)])
                stg = RR([sb(ph, f"stg{i}", [128, 512], BF16) for i in range(6)])
                vst = RR([sb(ph, f"vst{i}", [128, 8, 65], BF16) for i in range(2)])
                for t in vst.l: MS("dve", t[:], 1.0, [t])
                qset = RR([(sb(ph, f"qb{i}", [128, 512], F32), sb(ph, f"qs{i}", [128, 512], BF16), sb(ph, f"qr{i}", [128, 512], F32)) for i in range(4)])
                sg = RR([sb(ph, f"sg{i}", [128, 512], F32) for i in range(2)])
                def inb(g): return spc(l, "in_b", g, 1)
                Xs = [S.res(f"X{k}") for k in range(8)]
                def loadX(i):
                    t0, n, j = SUBT[i]
                    S.dma("sp", X[:, :, 0:n], Xsrc[:, t0:t0 + n].rearrange("(k p) t -> p k t", p=128), [], Xs)
                def square(i):
                    t0, n, j = SUBT[i]
                    S.op("act", "activation", dict(out=sq[:, :, 0:n], in_=X[:, :, 0:n], func=AF.Square), Xs, [sq.res])
                def prologue(i, h):
                    t0, n, j = SUBT[i]
                    for k in range(8):
                        MM(pss[:, 0:n], onesb[:], sq[:, k, 0:n], k == 0, k == 7, [onesb, sq], [pss])
                    rstd_from_ss(pss, n, rs, 1.0 / D)
                    for k in range(8):
                        S.op("dve", "tensor_tensor", dict(out=X[:, k, 0:n], in0=X[:, k, 0:n], in1=rs[:, 0:n], op=ALU.mult), [Xs[k], rs.res], [Xs[k]])
                        S.op("act", "activation", dict(out=h[:, k, 0:n], in_=X[:, k, 0:n], func=AF.Identity, scale=gs1[:, l, j, k:k + 1], bias=modv[:, l, j, k:k + 1]),
                             [Xs[k], gs1.res, modv.res], [hres[id(h)][k]])
                NT = len(SUBT)
                ALLW = ("U", "Q", "K", "V", "RX", "RG", "G3")
                def wants(i):
                    if l < DEPTH - 1: return ALLW
                    if i == 0: return ("K", "V", "RX")
                    if i <= NOWN: return ALLW
                    if i == NOWN + 1: return ("U", "K", "V", "RX")
                    return ("RX",)
                def main(i, h, sq_next):
                    t0, n, j = SUBT[i]; want = wants(i)
                    def zmm(ps, col0, n):
                        for k in range(8):
                            MM(ps[:, 0:n], W[:, k, col0:col0 + 128], h[:, k, 0:n], k == 0, k == 7, [Ws[col0 // 512], hres[id(h)][k]], [ps])
                    for g in range(4 if "U" in want else 0):
                        pa = pz(); zmm(pa, O_A + g * 128, n)
                        pg = pz(); zmm(pg, O_GA + g * 128, n)
                        s_ = sg(); st = stg()
                        ACT(s_[:, 0:n], pg[:, 0:n], AF.Sigmoid, [pg, spt], [s_], bias=inb(4 + g), scale=1.0)
                        STT(st[:, 0:n], pa[:, 0:n], inb(g), s_[:, 0:n], ALU.add, ALU.mult, [pa, s_, spt], [st])
                        DMA("sp", U[g * 128:(g + 1) * 128, t0:t0 + n], st[:, 0:n], rd=[st])
                    pend = []
                    def finish(b_, s2, r_, st, gname, extra, dst, g):
                        MM(pq[:, 0:n], blk64[:], s2[:, 0:n], True, True, [blk64, s2], [pq])
                        rstd_from_ss(pq, n, r_, 1.0 / 64)
                        STT(b_[:, 0:n], b_[:, 0:n], spc(l, gname, 0, 1), r_[:, 0:n], ALU.mult, ALU.mult, [b_, r_, spt], [b_])
                        TS("pool", st[:, 0:n], b_[:, 0:n], extra, 1.0, ALU.mult, ALU.mult, [b_], [st])
                        DMA("sp", dst[g * 128:(g + 1) * 128, t0:t0 + n], st[:, 0:n], rd=[st])
                    for col0, gname, dst, extra in ((O_Q, "qg", QN, 0.125), (O_K, "kg", KN, 1.0)):
                        if ("Q" if col0 == O_Q else "K") not in want: continue
                        for g in range(4):
                            pa = pz(); zmm(pa, col0 + g * 128, n)
                            b_, s2, r_ = qset(); st = stg()
                            bias_ap = inb(col0 // 128 + g)
                            ACT(b_[:, 0:n], pa[:, 0:n], AF.Identity, [pa, spt], [b_], bias=bias_ap, scale=1.0)
                            ACT(s2[:, 0:n], pa[:, 0:n], AF.Square, [pa, spt], [s2], bias=bias_ap, scale=1.0)
                            pend.append((b_, s2, r_, st, gname, extra, dst, g))
                            if len(pend) > 2: finish(*pend.pop(0))
                    for tb in range(n // 128 if "V" in want else 0):
                        pa = pz()
                        for k in range(8):
                            MM(pa[:, :], h[:, k, tb * 128:(tb + 1) * 128], W[:, k, O_V:O_V + 512], k == 0, k == 7, [Ws[O_V // 512], hres[id(h)][k]], [pa])
                        if pend: finish(*pend.pop(0))
                        vt = vst()
                        TT_("dve", vt[:, :, 0:64], pa[:].rearrange("p (h d) -> p h d", h=8), inbv[:].rearrange("p (h d) -> p h d", h=8), ALU.add, [pa, inbv], [vt])
                        DMA("sp", V[t0 + tb * 128:t0 + (tb + 1) * 128, :], vt[:].rearrange("p h d -> p (h d)"), rd=[vt])
                    while pend: finish(*pend.pop(0))
                    if sq_next is not None: square(sq_next)
                    for wn_, col0, func, dst, ng in (("RX", O_RX, AF.Identity, RX, 4), ("RG", O_RG, AF.Gelu_apprx_tanh, RG, 4), ("G3", O_GCV, AF.Sigmoid, G3, 24)):
                        if wn_ not in want: continue
                        for g in range(ng):
                            pa = pz(); zmm(pa, col0 + g * 128, n)
                            st = stg()
                            ACT(st[:, 0:n], pa[:, 0:n], func, [pa, spt], [st], bias=inb(col0 // 128 + g), scale=1.0)
                            DMA("sp", dst[g * 128:(g + 1) * 128, t0:t0 + n], st[:, 0:n], rd=[st])
                seq_ = list(range(NT)) if l < DEPTH - 1 else [0] + list(range(NOWN + 2, NT)) + [NOWN + 1] + list(range(1, NOWN + 1))
                loadX(seq_[0]); square(seq_[0]); prologue(seq_[0], hh[0]); loadX(seq_[1]); square(seq_[1])
                for q_ in range(NT):
                    if q_ + 1 < NT:
                        prologue(seq_[q_ + 1], hh[(q_ + 1) % 2])
                        if q_ + 2 < NT: loadX(seq_[q_ + 2])
                    main(seq_[q_], hh[q_ % 2], seq_[q_ + 2] if q_ + 2 < NT else None)
                S.barrier()

        def phase2_conv(l):
            with contextlib.ExitStack() as ph:
                dg = sb(ph, "dg", [128, 4, 31, 128], BF16)
                for g in range(4):
                    for k in range(31):
                        TS("dve", dg[:, g, k, :], identb[:], spc(l, "cdw", g * 31 + k, 1), None, ALU.mult, None, [identb, spt], [dg])
                upad = sb(ph, "upad", [128, 4, SEQ + 30], BF16)
                py = [pst(ph, f"py{g}") for g in range(4)]
                pm = pst(ph, "pm"); pv = pst(ph, "pv")
                yf = sb(ph, "yf", [128, 4, 512], F32); yb = sb(ph, "yb", [128, 4, 512], BF16); ysq = sb(ph, "ysq", [128, 4, 512], BF16)
                mean = sb(ph, "mean", [128, 512], F32); var = sb(ph, "var", [128, 512], F32); m2 = sb(ph, "m2", [128, 512], F32)
                tt = RR([sb(ph, f"ctt{i}", [128, 512], F32) for i in range(2)])
                stg = RR([sb(ph, f"cst{i}", [128, 512], BF16) for i in range(4)])
                seqs = [(CTX, SEQ, False), (0, CTX, False)] if l < DEPTH - 1 else [(CTX, HALF, True)]
                for (s0, Ls, rhalo) in seqs:
                    for g in range(4):
                        MS("pool", upad[:, g, 0:15], 0.0, [upad])
                        if rhalo:
                            DMA("sp", upad[:, g, 15:30 + Ls], U[g * 128:(g + 1) * 128, s0:s0 + Ls + 15], wr=[upad])
                        else:
                            MS("pool", upad[:, g, 15 + Ls:30 + Ls], 0.0, [upad])
                            DMA("sp", upad[:, g, 15:15 + Ls], U[g * 128:(g + 1) * 128, s0:s0 + Ls], wr=[upad])
                    for t in range(0, Ls, 512):
                        n = min(512, Ls - t)
                        for g in range(4):
                            for k in range(31):
                                MM(py[g][:, 0:n], dg[:, g, k, :], upad[:, g, t + k:t + k + n], k == 0, k == 30, [dg, upad], [py[g]])
                            ACT(yf[:, g, 0:n], py[g][:, 0:n], AF.Identity, [py[g], spt], [yf], bias=spc(l, "cdb", g), scale=1.0)
                            ACT(ysq[:, g, 0:n], py[g][:, 0:n], AF.Square, [py[g], spt], [ysq], bias=spc(l, "cdb", g), scale=1.0)
                            CP("pool", yb[:, g, 0:n], yf[:, g, 0:n], [yf], [yb])
                        for g in range(4):
                            MM(pm[:, 0:n], onesb[:], yb[:, g, 0:n], g == 0, g == 3, [onesb, yb], [pm])
                        for g in range(4):
                            MM(pv[:, 0:n], onesb[:], ysq[:, g, 0:n], g == 0, g == 3, [onesb, ysq], [pv])
                        ACT(mean[:, 0:n], pm[:, 0:n], AF.Identity, [pm], [mean], scale=1.0 / 512, bias=0.0)
                        TT_("dve", m2[:, 0:n], mean[:, 0:n], mean[:, 0:n], ALU.mult, [mean], [m2])
                        STT(var[:, 0:n], pv[:, 0:n], 1.0 / 512, m2[:, 0:n], ALU.mult, ALU.subtract, [pv, m2], [var])
                        ACT(var[:, 0:n], var[:, 0:n], AF.Ln, [var], [var], bias=EPS, scale=1.0)
                        ACT(var[:, 0:n], var[:, 0:n], AF.Exp, [var], [var], scale=-0.5)
                        for g in range(4):
                            t_ = tt(); st = stg()
                            TT_("dve", t_[:, 0:n], yf[:, g, 0:n], mean[:, 0:n], ALU.subtract, [yf, mean], [t_])
                            TT_("dve", t_[:, 0:n], t_[:, 0:n], var[:, 0:n], ALU.mult, [t_, var], [t_])
                            ACT(st[:, 0:n], t_[:, 0:n], AF.Silu, [t_, spt], [st], scale=spc(l, "clg", g), bias=spc(l, "clb", g))
                            DMA("sp", UC[g * 128:(g + 1) * 128, s0 + t:s0 + t + n], st[:, 0:n], rd=[st])
                S.barrier()

        def rnn_gen(l, ph):
            if True:
                rb = sb(ph, "rb", [128, 2, 2, 4, 128], BF16)
                DMA("pool", rb[:], rnnblk_d[l].rearrange("d w g k j -> k d w g j"), wr=[rb])
                hf = sb(ph, "hf", [128, TT], BF16)
                carry = sb(ph, "carry", [128, 1], F32)
                rdg = sb(ph, "rdg", [128, 8, 4, 128], BF16)
                for dgi_ in range(8):
                    for jj in range(4):
                        TS("dve", rdg[:, dgi_, jj, :], identb[:], spc(l, "rcw", dgi_ * 4 + jj), None, ALU.mult, None, [identb, spt], [rdg])
                pcv = RR([pst(ph, f"pcv{i}") for i in range(2)])
                NS = 2048
                def mk(i):
                    return dict(rxp=sb(ph, f"rxp{i}", [128, NS + 3], BF16), ucf=sb(ph, f"ucf{i}", [128, NS], F32), uc16=sb(ph, f"uc16{i}", [128, NS], BF16),
                                r=sb(ph, f"r{i}", [128, NS], F32), ig=sb(ph, f"ig{i}", [128, NS], F32), m=sb(ph, f"m{i}", [128, NS], F32),
                                bb=sb(ph, f"bb{i}", [128, NS], F32), hs=sb(ph, f"hs{i}", [128, NS], F32), rg=sb(ph, f"rg{i}", [128, NS], BF16),
                                st=sb(ph, f"rst{i}", [128, NS], BF16))
                sets = RR([mk(0), mk(1)])
                ppr = RR([pst(ph, f"ppr{i}") for i in range(3)]); ppi = RR([pst(ph, f"ppi{i}") for i in range(3)])
                NSEG = SEQ // NS
                lat = [(CTX + NS * i, NS) for i in range(NSEG)]
                steps = []
                for g in range(4):
                    for d in range(2):
                        half = (l == DEPTH - 1)
                        segs = [(0, CTX, True, True, not half)] + [(s0, n, i == 0, i == NSEG - 1, (not half) or i < NSEG // 2) for i, (s0, n) in enumerate(lat)]
                        if d == 1:
                            segs = [segs[0]] + segs[:0:-1]
                        elif half:
                            segs = segs[:1 + NSEG // 2]
                        for si, sg_ in enumerate(segs):
                            steps.append((g, d, si == 0) + sg_)
                def stageA(step):
                    g, d, first, s0, n, at_start, at_end, own = step
                    dgi = d * 4 + g
                    B = sets()
                    rxp, ucf, uc16, r, ig, m, rgt = (B[k] for k in ("rxp", "ucf", "uc16", "r", "ig", "m", "rg"))
                    rows = slice(g * 128, (g + 1) * 128)
                    if d == 0:
                        if at_start:
                            MS("pool", rxp[:, 0:3], 0.0, [rxp]); DMA("sp", rxp[:, 3:3 + n], RX[rows, s0:s0 + n], wr=[rxp])
                        else:
                            DMA("sp", rxp[:, 0:3 + n], RX[rows, s0 - 3:s0 + n], wr=[rxp])
                        offs = [0, 1, 2, 3]
                    else:
                        if at_end:
                            MS("pool", rxp[:, n:n + 3], 0.0, [rxp]); DMA("sp", rxp[:, 0:n], RX[rows, s0:s0 + n], wr=[rxp])
                        else:
                            DMA("sp", rxp[:, 0:n + 3], RX[rows, s0:s0 + n + 3], wr=[rxp])
                        offs = [3, 2, 1, 0]
                    if d == 1 and own:
                        DMA("sp", rgt[:, 0:n], RG[rows, s0:s0 + n], wr=[rgt])
                    for t in range(0, n, 512):
                        nn = min(512, n - t)
                        pc = pcv()
                        for jj in range(4):
                            MM(pc[:, 0:nn], rdg[:, dgi, jj, :], rxp[:, t + offs[jj]:t + offs[jj] + nn], jj == 0, jj == 3, [rdg, rxp], [pc])
                        ACT(uc16[:, t:t + nn], pc[:, 0:nn], AF.Identity, [pc, spt], [uc16], bias=spc(l, "rcb", dgi), scale=1.0)
                        ACT(ucf[:, t:t + nn], pc[:, 0:nn], AF.Identity, [pc, spt], [ucf], bias=spc(l, "rcb", dgi), scale=1.0)
                    for t in range(0, n, 512):
                        nn = min(512, n - t)
                        pr = ppr(); pi = ppi()
                        MM(pr[:, 0:nn], rb[:, d, 0, g, :], uc16[:, t:t + nn], True, True, [rb, uc16], [pr])
                        MM(pi[:, 0:nn], rb[:, d, 1, g, :], uc16[:, t:t + nn], True, True, [rb, uc16], [pi])
                        ACT(r[:, t:t + nn], pr[:, 0:nn], AF.Sigmoid, [pr, spt], [r], bias=spc(l, "rba", dgi), scale=1.0)
                        ACT(ig[:, t:t + nn], pi[:, 0:nn], AF.Sigmoid, [pi, spt], [ig], bias=spc(l, "rbx", dgi), scale=1.0)
                    ACT(m[:, 0:n], r[:, 0:n], AF.Exp, [r, c16], [m], scale=c16[:, l, dgi:dgi + 1])
                    ACT(r[:, 0:n], r[:, 0:n], AF.Exp, [r, c8], [r], scale=c8[:, l, dgi:dgi + 1])
                    ACT(m[:, 0:n], m[:, 0:n], AF.Sqrt, [m], [m], scale=-1.0, bias=1.0)
                    return B
                def stageB(step, B):
                    g, d, first, s0, n, at_start, at_end, own = step
                    ucf, r, ig, m, bb, hs, rgt, st = (B[k] for k in ("ucf", "r", "ig", "m", "bb", "hs", "rg", "st"))
                    rows = slice(g * 128, (g + 1) * 128)
                    TT_("dve", bb[:, 0:n], ig[:, 0:n], ucf[:, 0:n], ALU.mult, [ig, ucf], [bb])
                    TT_("dve", bb[:, 0:n], bb[:, 0:n], m[:, 0:n], ALU.mult, [bb, m], [bb])
                    init = 0.0 if first else carry[:, 0:1]
                    if d == 0:
                        S.op("dve", "tensor_tensor_scan", dict(out=hs[:, 0:n], data0=r[:, 0:n], data1=bb[:, 0:n], initial=init, op0=ALU.mult, op1=ALU.add),
                             R([r, bb, carry]), R([hs]))
                        CP("dve", carry[:, 0:1], hs[:, n - 1:n], [hs], [carry])
                        if own:
                            ACT(hf[:, s0:s0 + n], hs[:, 0:n], AF.Identity, [hs], [hf])
                    else:
                        S.op("dve", "tensor_tensor_scan", dict(out=hs[:, 0:n][:, ::-1], data0=r[:, 0:n][:, ::-1], data1=bb[:, 0:n][:, ::-1],
                             initial=init, op0=ALU.mult, op1=ALU.add), R([r, bb, carry]), R([hs]))
                        CP("dve", carry[:, 0:1], hs[:, 0:1], [hs], [carry])
                        if own:
                            TT_("pool", hs[:, 0:n], hs[:, 0:n], hf[:, s0:s0 + n], ALU.add, [hs, hf], [hs])
                            TT_("pool", st[:, 0:n], hs[:, 0:n], rgt[:, 0:n], ALU.mult, [hs, rgt], [st])
                            DMA("sp", RN[rows, s0:s0 + n], st[:, 0:n], rd=[st])
                yield len(steps)
                cur = stageA(steps[0])
                for si in range(len(steps)):
                    nxt_ = stageA(steps[si + 1]) if si + 1 < len(steps) else None
                    stageB(steps[si], cur)
                    cur = nxt_
                    yield 1

        def na_gen(l, ph):
            if True:
                def mkin(i):
                    return (sb(ph, f"kn{i}", [128, TT], BF16), [sb(ph, f"qz{i}_{h}", [128, TT], BF16) for h in range(2)],
                            sb(ph, f"vs{i}", [128, TT // 128, 130], BF16), sb(ph, f"nb{i}", [128, 2, ntypes, 128], BF16))
                insets = [mkin(0), mkin(1)]
                for (_, qz_, _, _) in insets:
                    for h in range(2): MS("pool", qz_[h][:], 0.0, [qz_[h]])
                def load_in(hg):
                    kn, qz, vs, nb = insets[hg % 2]
                    rows = slice(hg * 128, (hg + 1) * 128)
                    DMA("sp", kn[:], KN[rows, :], wr=[kn])
                    for h in range(2):
                        DMA("sp", qz[h][h * 64:(h + 1) * 64, :], QN[hg * 128 + h * 64:hg * 128 + (h + 1) * 64, :], wr=[qz[h]])
                    DMA("sp", vs[:], V.rearrange("(c p) f -> p c f", p=128)[:, :, hg * 130:(hg + 1) * 130], wr=[vs])
                    for h in range(2):
                        DMA("pool", nb[:, h], nab_d[l, 2 * hg + h].rearrange("t k q -> k t q"), wr=[nb])
                naT = sb(ph, "naT", [128, TT], BF16)
                psA = RR([pst(ph, f"psA{i}") for i in range(2)]); psB = RR([pst(ph, f"psB{i}") for i in range(2)])
                po = RR([pst(ph, f"po{i}") for i in range(2)])
                ptr = pst(ph, "ptr", (128, 1024), BF16)
                pT = RR([sb(ph, f"pT{i}", [128, 8, 128], BF16) for i in range(3)])
                nat = RR([sb(ph, f"nat{i}", [128, 128], BF16) for i in range(2)])
                rc = RR([sb(ph, f"rc{i}", [128, 2], F32) for i in range(2)])
                yield 4 * 2 * ((64 if l < DEPTH - 1 else HALF // 128) + (2 if l < DEPTH - 1 else 0))
                for hg in range(4):
                    rows = slice(hg * 128, (hg + 1) * 128)
                    kn, qz, vs, nb = insets[hg % 2]
                    if hg == 0: load_in(0)
                    if hg + 1 < 4: load_in(hg + 1)
                    units = [(CTX + 128 * p, [(2 + c, tid) for (c, tid) in naplan[p]] + [(0, None), (1, None)]) for p in range(64 if l < DEPTH - 1 else HALF // 128)]
                    if l < DEPTH - 1:
                        units += [(128 * pc, [(0, None), (1, None)]) for pc in range(2)]
                    items = [(ui, h) for ui in range(len(units)) for h in range(2)]
                    st_ = {}
                    def qk(idx):
                        ui, h = items[idx]; q0, chunks = units[ui]
                        pa = psA(); pb = psB(); pt = pT(); nch = len(chunks)
                        for i, (vc, tid) in enumerate(chunks):
                            bank = pa if i < 4 else pb
                            oap = bank[:, (i % 4) * 128:(i % 4 + 1) * 128]
                            MM(oap, kn[:, vc * 128:(vc + 1) * 128], qz[h][:, q0:q0 + 128], True, True, [kn, qz[h]], [bank])
                        loc = [tid for (_, tid) in chunks if tid is not None]
                        if loc:
                            assert loc == list(range(loc[0], loc[0] + len(loc))) and all(t_ is not None for (_, t_) in chunks[:len(loc)])
                            na4 = min(len(loc), 4)
                            TT_("dve", pa[:, 0:na4 * 128], pa[:, 0:na4 * 128], nb[:, h, loc[0]:loc[0] + na4, :].rearrange("p t q -> p (t q)"), ALU.add, [pa, nb], [pa])
                            if len(loc) > 4:
                                TT_("dve", pb[:, 0:128], pb[:, 0:128], nb[:, h, loc[4], :], ALU.add, [pb, nb], [pb])
                        na_ = min(nch, 4)
                        ACT(pt[:, 0:na_, :], pa[:, 0:na_ * 128].rearrange("p (c q) -> p c q", q=128), AF.Exp, [pa], [pt])
                        if nch > 4:
                            ACT(pt[:, 4:nch, :], pb[:, 0:(nch - 4) * 128].rearrange("p (c q) -> p c q", q=128), AF.Exp, [pb], [pt])
                        st_[idx] = pt
                    def pv(idx):
                        ui, h = items[idx]; q0, chunks = units[ui]; nch = len(chunks)
                        if h == 0:
                            st_["o"] = (po(), nat(), rc())
                        o_ps, nt, rc_ = st_["o"]; pt = st_.pop(idx)
                        for i, (vc, tid) in enumerate(chunks):
                            MM(o_ps[:, h * 65:(h + 1) * 65], pt[:, i, :], vs[:, vc, h * 65:(h + 1) * 65], i == 0, i == nch - 1, [pt, vs], [o_ps])
                        if h == 1:
                            S.op("dve", "reciprocal", dict(out=rc_[:, 0:2], in_=o_ps[:, 64:130:65]), R([o_ps]), R([rc_]))
                            for hh_ in range(2):
                                TS("dve", nt[:, hh_ * 64:(hh_ + 1) * 64], o_ps[:, hh_ * 65:hh_ * 65 + 64], rc_[:, hh_:hh_ + 1], None, ALU.mult, None, [o_ps, rc_], [nt])
                            S.op("pe", "transpose", dict(out=ptr[:, 0:128], in_=nt[:], identity=identb[:]), R([nt, identb]), R([ptr]))
                            ACT(naT[:, q0:q0 + 128], ptr[:, 0:128], AF.Identity, [ptr], [naT])
                    qk(0)
                    for idx in range(len(items)):
                        if idx + 1 < len(items): qk(idx + 1)
                        pv(idx)
                        yield 1
                    DMA("sp", NA[rows, :], naT[:], rd=[naT])

        def phase2_rnn_na(l):
            for gen in (rnn_gen, na_gen):
                with contextlib.ExitStack() as ph:
                    for _ in gen(l, ph): pass
                    S.barrier()

        def phase3a(l, Xsrc):
            with contextlib.ExitStack() as ph:
                pw = [sb(ph, f"pw{i}", [128, 4, D], BF16) for i in range(3)]
                for i, src in enumerate((conv_pw, na_out, rnn_out)):
                    DMA("pool", pw[i][:], src[l].rearrange("(k p) n -> p k n", p=128), wr=[pw[i]])
                ow = sb(ph, "ow", [128, 8, D], BF16)
                for s in range(2):
                    DMA("pool", ow[:, :, s * 512:(s + 1) * 512], out_w[l].rearrange("(k p) n -> p k n", p=128)[:, :, s * 512:(s + 1) * 512], wr=[ow])
                rw = sb(ph, "rw", [128, 8, NE], F32); rbias = sb(ph, "rbias", [128, 4 * NE], F32)
                DMA("sp", rw[:], router_w.rearrange("(k p) e -> p k e", p=128), wr=[rw]); DMA("sp", rbias[:], rbias_d[:, :], wr=[rbias])
                def mk(i):
                    return dict(br=[sb(ph, f"br{i}_{b}", [128, 4, 512], BF16) for b in range(3)], g3=sb(ph, f"g3{i}", [128, 24, 512], BF16),
                                x=sb(ph, f"x{i}", [128, 8, 512], F32))
                sets = RR([mk(0), mk(1)])
                mT = sb(ph, "mT", [128, 8, 512], BF16)
                pbr = RR([pst(ph, f"pb{b}") for b in range(5)])
                py = RR([pst(ph, f"p3y{i}") for i in range(2)])
                pss = pst(ph, "pss3"); prt = pss; ptg = pss
                tsr = RR([[sb(ph, f"t3_{i}_{b}", [128, 512], F32) for b in range(3)] for i in range(2)])
                sq = sb(ph, "sq3", [128, 8, 512], BF16); hn = sb(ph, "hn3", [128, 8, 512], F32); rs = sb(ph, "rs3", [128, 512], F32)
                h2o = sb(ph, "h2o", [128, 8, 512], BF16); gt = sb(ph, "gt3", [NE, 512], F32)
                hns = [S.res(f"hn{k}") for k in range(8)]
                def sm(name, w): return sb(ph, name, [128, w], F32)
                sc = sm("sc", 64); sl = sm("sl", 64); s2 = sm("s2", 64); msk = sm("msk", 64); wt = sm("wt", 64); gate = sm("gate", 64)
                m1 = sm("m1", 16); m2 = sm("m2", 16); gsc = sm("gsc", 16); goh = sm("goh", 16); gm = sm("gm", 4); ws = sm("ws", 4)
                def bc(ap, shape):
                    return ap.unsqueeze(2).broadcast_to(shape)
                mysubs = [s for s in SUBT if not (l == DEPTH - 1 and (s[2] == 1 or s[0] >= CTX + HALF))]
                def front(t0, n, j):
                    B = sets(); X_ = B["x"]
                    for b, src in enumerate((UC, NA, RN)):
                        DMA("sp", B["br"][b][:, :, 0:n], src[:, t0:t0 + n].rearrange("(g p) t -> p g t", p=128), wr=[B["br"][b]])
                    DMA("sp", B["g3"][:, :, 0:n], G3[:, t0:t0 + n].rearrange("(g p) t -> p g t", p=128), wr=[B["g3"]])
                    DMA("sp", X_[:, :, 0:n], Xsrc[:, t0:t0 + n].rearrange("(k p) t -> p k t", p=128), wr=[X_])
                    for og in range(8):
                        ts_ = tsr()
                        for b in range(3):
                            p_ = pbr()
                            for k in range(4):
                                MM(p_[:, 0:n], pw[b][:, k, og * 128:(og + 1) * 128], B["br"][b][:, k, 0:n], k == 0, k == 3, [pw[b], B["br"][b]], [p_])
                            TT_("dve", ts_[b][:, 0:n], p_[:, 0:n], B["g3"][:, b * 8 + og, 0:n], ALU.mult, [p_, B["g3"]], [ts_[b]])
                        TT_("pool", ts_[0][:, 0:n], ts_[0][:, 0:n], ts_[1][:, 0:n], ALU.add, [ts_[0], ts_[1]], [ts_[0]])
                        TT_("pool", mT[:, og, 0:n], ts_[0][:, 0:n], ts_[2][:, 0:n], ALU.add, [ts_[0], ts_[2]], [mT])
                    for og in range(8):
                        p_ = py()
                        for k in range(8):
                            MM(p_[:, 0:n], ow[:, k, og * 128:(og + 1) * 128], mT[:, k, 0:n], k == 0, k == 7, [ow, mT], [p_])
                        STT(X_[:, og, 0:n], p_[:, 0:n], modv[:, l, j, 16 + og:17 + og], X_[:, og, 0:n], ALU.mult, ALU.add, [p_, modv, X_], [X_])
                    DMA("sp", XM[:, t0:t0 + n].rearrange("(k p) t -> p k t", p=128), X_[:, :, 0:n], rd=[X_])
                    return X_
                def tail(t0, n, j, X_):
                    nb = n // 128
                    ACT(sq[:, :, 0:n], X_[:, :, 0:n], AF.Square, [X_], [sq])
                    for k in range(8):
                        MM(pss[:, 0:n], onesb[:], sq[:, k, 0:n], k == 0, k == 7, [onesb, sq], [pss])
                    rstd_from_ss(pss, n, rs, 1.0 / D)
                    for k in range(8):
                        TT_("dve", hn[:, k, 0:n], X_[:, k, 0:n], rs[:, 0:n], ALU.mult, [X_, rs], [hns[k]])
                        ACT(hn[:, k, 0:n], hn[:, k, 0:n], AF.Identity, [hns[k], gs2, modv], [hns[k]], scale=gs2[:, l, j, k:k + 1], bias=modv[:, l, j, 24 + k:25 + k])
                        CP("pool", h2o[:, k, 0:n], hn[:, k, 0:n], [hns[k]], [h2o])
                    DMA("sp", H2[:, t0:t0 + n].rearrange("(k p) t -> p k t", p=128), h2o[:, :, 0:n], rd=[h2o])
                    for tb in range(nb):
                        for k in range(8):
                            MM(prt[:, tb * NE:(tb + 1) * NE], hn[:, k, tb * 128:(tb + 1) * 128], rw[:, k, :], k == 0, k == 7, [hns[k], rw], [prt])
                    w = nb * NE; g4 = nb * 4
                    def v3(t_, inner): return t_[:, 0:w].rearrange("p (a e) -> p a e", e=inner)
                    ACT(sc[:, 0:w], prt[:, 0:w], AF.Sigmoid, [prt], [sc])
                    TT_("dve", sl[:, 0:w], sc[:, 0:w], rbias[:, 0:w], ALU.add, [sc, rbias], [sl])
                    S.op("dve", "tensor_reduce", dict(out=m1[:, 0:g4], in_=v3(sl, 4), axis=AX.X, op=ALU.max), R([sl]), R([m1]))
                    TT_("dve", v3(s2, 4), v3(sl, 4), bc(m1[:, 0:g4], [128, g4, 4]), ALU.is_equal, [sl, m1], [s2])
                    STT(s2[:, 0:w], s2[:, 0:w], -1e9, sl[:, 0:w], ALU.mult, ALU.add, [s2, sl], [s2])
                    S.op("dve", "tensor_reduce", dict(out=m2[:, 0:g4], in_=v3(s2, 4), axis=AX.X, op=ALU.max), R([s2]), R([m2]))
                    TT_("dve", gsc[:, 0:g4], m1[:, 0:g4], m2[:, 0:g4], ALU.add, [m1, m2], [gsc])
                    S.op("dve", "tensor_reduce", dict(out=gm[:, 0:nb], in_=gsc[:, 0:g4].rearrange("p (a e) -> p a e", e=4), axis=AX.X, op=ALU.max), R([gsc]), R([gm]))
                    TT_("dve", goh[:, 0:g4].rearrange("p (a e) -> p a e", e=4), gsc[:, 0:g4].rearrange("p (a e) -> p a e", e=4), bc(gm[:, 0:nb], [128, nb, 4]), ALU.is_equal, [gsc, gm], [goh])
                    TT_("dve", v3(msk, 4), v3(sl, 4), bc(m2[:, 0:g4], [128, g4, 4]), ALU.is_ge, [sl, m2], [msk])
                    TT_("dve", v3(msk, 4), v3(msk, 4), bc(goh[:, 0:g4], [128, g4, 4]), ALU.mult, [msk, goh], [msk])
                    TT_("dve", wt[:, 0:w], sc[:, 0:w], msk[:, 0:w], ALU.mult, [sc, msk], [wt])
                    S.op("dve", "tensor_reduce", dict(out=ws[:, 0:nb], in_=v3(wt, NE), axis=AX.X, op=ALU.add), R([wt]), R([ws]))
                    S.op("dve", "reciprocal", dict(out=ws[:, 0:nb], in_=ws[:, 0:nb]), R([ws]), R([ws]))
                    TT_("dve", v3(gate, NE), v3(wt, NE), bc(ws[:, 0:nb], [128, nb, NE]), ALU.mult, [wt, ws], [gate])
                    for tb in range(nb):
                        S.op("pe", "transpose", dict(out=ptg[0:NE, tb * 128:(tb + 1) * 128], in_=gate[:, tb * NE:(tb + 1) * NE], identity=ident[:]), R([gate, ident]), R([ptg]))
                    CP("dve", gt[:, 0:n], ptg[0:NE, 0:n], [ptg], [gt])
                    DMA("sp", GT[:, t0:t0 + n], gt[:, 0:n], rd=[gt])
                xs_ = front(*mysubs[0])
                for i_, s_ in enumerate(mysubs):
                    nx_ = front(*mysubs[i_ + 1]) if i_ + 1 < len(mysubs) else None
                    tail(*s_, xs_)
                    xs_ = nx_
                S.barrier()

        def phase3b(l, final):
            with contextlib.ExitStack() as ph:
                SMAX = 1280
                xm = sb(ph, "xm", [128, 8, SMAX], F32); xms = [S.res(f"xm{k}") for k in range(8)]
                hg = RR([(sb(ph, f"h2b{i}", [128, 8, SMAX], BF16), sb(ph, f"gT{i}", [NE, SMAX], F32)) for i in range(2)])
                selt = sb(ph, "selt", [NE, NE * 128], F32)
                DMA("sp", selt[:], sel_d[:, :], wr=[selt])
                wset = RR([(sb(ph, f"w1b{i}", [128, 8, DE], BF16), sb(ph, f"w3b{i}", [128, 8, DE], BF16), sb(ph, f"w2b{i}", [128, 4, D], BF16)) for i in range(2)])
                pg = RR([pst(ph, f"pg{i}") for i in range(2)])
                p1 = RR([pst(ph, f"p1_{i}") for i in range(2)]); p3 = RR([pst(ph, f"p3_{i}") for i in range(2)]); po = RR([pst(ph, f"po2{i}") for i in range(2)])
                gbs = RR([sb(ph, f"gb{i}", [128, 512], F32) for i in range(4)])
                s1 = RR([sb(ph, f"s1_{i}", [128, 512], F32) for i in range(2)]); tt = RR([sb(ph, f"tt_{i}", [128, 512], F32) for i in range(2)])
                he = RR([sb(ph, f"he{i}", [128, 4, 512], BF16) for i in range(2)])
                subs = [s for s in SUBT if not (l == DEPTH - 1 and (s[2] == 1 or s[0] >= CTX + HALF))]
                supers = [subs[0:3]] + [subs[i:i + 2] for i in range(3, len(subs), 2)] if l < DEPTH - 1 else [subs[i:i + 2] for i in range(0, len(subs), 2)]
                def load_hg(sup):
                    c0 = sup[0][0]; Sn = sum(s[1] for s in sup); h2b, gT = hg()
                    DMA("sp", h2b[:, :, 0:Sn], H2[:, c0:c0 + Sn].rearrange("(k p) t -> p k t", p=128), wr=[h2b])
                    DMA("sp", gT[:, 0:Sn], GT[:, c0:c0 + Sn], wr=[gT])
                    return h2b, gT
                nxt_hg = load_hg(supers[0])
                wcache = {}
                def weights(key, e):
                    if key not in wcache:
                        w1b, w3b, w2b = wset()
                        for half in range(2):
                            DMA("pool", w1b[:, half * 4:(half + 1) * 4, :], w1_d[l, e].rearrange("(k p) n -> p k n", p=128)[:, half * 4:(half + 1) * 4, :], wr=[w1b])
                            DMA("pool", w3b[:, half * 4:(half + 1) * 4, :], w3_d[l, e].rearrange("(k p) n -> p k n", p=128)[:, half * 4:(half + 1) * 4, :], wr=[w3b])
                            DMA("pool", w2b[:, half * 2:(half + 1) * 2, :], w2_d[l, e].rearrange("(k p) n -> p k n", p=128)[:, half * 2:(half + 1) * 2, :], wr=[w2b])
                        wcache[key] = (w1b, w3b, w2b)
                    return wcache[key]
                for si, sup in enumerate(supers):
                    c0 = sup[0][0]; Sn = sum(s[1] for s in sup)
                    h2b, gT = nxt_hg
                    for og in range(8):
                        S.dma("sp", xm[:, og, 0:Sn], XM[og * 128:(og + 1) * 128, c0:c0 + Sn], [], [xms[og]])
                    ulist = [(e, s_) for e in range(NE) for s_ in sup]
                    def gate_bc(ui):
                        e, (t0, n, j) = ulist[ui]; g_ = gbs()
                        DMA("sp", g_[:, 0:n], GT[e:e + 1, t0:t0 + n].broadcast_to([128, n]), wr=[g_])
                        return g_
                    def down(ui, he_):
                        e, (t0, n, j) = ulist[ui]; o = t0 - c0; w2b = weights((si, e), e)[2]
                        for og in range(8):
                            p_ = po()
                            for k in range(4):
                                MM(p_[:, 0:n], w2b[:, k, og * 128:(og + 1) * 128], he_[:, k, 0:n], k == 0, k == 3, [w2b, he_], [p_])
                            S.op("dve", "scalar_tensor_tensor", dict(out=xm[:, og, o:o + n], in0=p_[:, 0:n], scalar=modv[:, l, j, 40 + og:41 + og], in1=xm[:, og, o:o + n],
                                 op0=ALU.mult, op1=ALU.add), [p_.res, modv.res, xms[og]], [xms[og]])
                    gq = [gate_bc(0)] + ([gate_bc(1)] if len(ulist) > 1 else []); prev = None
                    for ui, (e, (t0, n, j)) in enumerate(ulist):
                        o = t0 - c0; w1b, w3b, w2b = weights((si, e), e)
                        he_ = he()
                        for fc in range(4):
                            p1_ = p1(); p3_ = p3()
                            for k in range(8):
                                MM(p1_[:, 0:n], w1b[:, k, fc * 128:(fc + 1) * 128], h2b[:, k, o:o + n], k == 0, k == 7, [w1b, h2b], [p1_])
                            for k in range(8):
                                MM(p3_[:, 0:n], w3b[:, k, fc * 128:(fc + 1) * 128], h2b[:, k, o:o + n], k == 0, k == 7, [w3b, h2b], [p3_])
                            s1_ = s1(); t_ = tt()
                            ACT(s1_[:, 0:n], p1_[:, 0:n], AF.Silu, [p1_], [s1_])
                            TT_("dve", t_[:, 0:n], s1_[:, 0:n], p3_[:, 0:n], ALU.mult, [s1_, p3_], [t_])
                            TT_("pool", he_[:, fc, 0:n], t_[:, 0:n], gq[0][:, 0:n], ALU.mult, [t_, gq[0]], [he_])
                        gq.pop(0)
                        if ui + 2 < len(ulist):
                            gq.append(gate_bc(ui + 2))
                        if prev is not None: down(*prev)
                        prev = (ui, he_)
                        if (ui == 0 or ulist[ui - 1][0] != e):
                            if e + 1 < NE:
                                weights((si, e + 1), e + 1)
                            elif si + 1 < len(supers):
                                weights((si + 1, 0), 0)
                        if ui == 0 and si + 1 < len(supers):
                            nxt_hg = load_hg(supers[si + 1])
                    down(*prev)
                    dst = outT[:, c0 - CTX:c0 - CTX + Sn] if final else XN[:, c0:c0 + Sn]
                    for og in range(8):
                        S.dma("sp", dst[og * 128:(og + 1) * 128, :], xm[:, og, 0:Sn], [xms[og]], [])
                S.barrier()

        order = ["p1", "conv", "rnn", "na", "p3a", "p3b"]
        def run_layer(l, Xsrc, stop=None):
            for name, fn in (("p1", lambda: (phase1(l, Xsrc, preW0 if l == 0 else None), w0stack.close() if l == 0 else None)), ("conv", lambda: phase2_conv(l)), ("na", lambda: phase2_rnn_na(l)), ("p3a", lambda: phase3a(l, Xsrc)), ("p3b", lambda: phase3b(l, l == DEPTH - 1))):
                fn()
                if stop == name:
                    return True
            return False
        if upto == "all":
            run_layer(0, xT); run_layer(1, XN)
        elif upto.startswith("only:"):
            {"conv": lambda: phase2_conv(0), "na": lambda: phase2_rnn_na(0)}[upto[5:]]()
        elif upto == "l0":
            run_layer(0, xT)
        else:
            run_layer(0, xT, stop=upto)
        for n_, ap in dump_out.items():
            S.dma("sp", ap, scr[n_])
        S.barrier()
        S.emit()
    return nc


def prep_shared(inp, rev=False):
    types, _ = na_geometry()
    sh = {}
    dsl = slice(None, None, -1) if rev else slice(None)
    sh["ident_in"] = np.eye(128, dtype=np.float32)
    sh["sp"] = np.stack([pack_small(inp, l, rev) for l in range(DEPTH)])
    sh["inbv"] = np.stack([np.ascontiguousarray(np.broadcast_to(inp["in_b"][l, O_V:O_V + 512], (128, 512))) for l in range(DEPTH)])
    sh["rbias"] = np.ascontiguousarray(np.broadcast_to(np.tile(inp["router_bias"], 4), (128, 4 * NE)))
    for k in ("mod_w", "in_w", "conv_pw_w", "na_out_w", "rnn_out_w", "out_w", "exp_w1", "exp_w3", "exp_w2", "router_w"):
        sh[k] = np.ascontiguousarray(inp[k], dtype=np.float32)
    blk = np.zeros((DEPTH, 2, 2, 4, 128, 128), np.float32)
    for l in range(DEPTH):
        for d in range(2):
            for wi, wn in enumerate(("rnn_wa", "rnn_wx")):
                w = inp[wn][l][dsl][d]
                for n in range(8):
                    g, o = n // 2, (n % 2) * 64
                    blk[l, d, wi, g, o:o + 64, o:o + 64] = w[n]
    sh["rnnblk"] = blk
    sh["nabias"] = np.stack([build_na_bias(inp["na_rpb"][l], rev) for l in range(DEPTH)])
    sel = np.zeros((NE, NE * 128), np.float32)
    for e in range(NE):
        sel[e, e * 128:(e + 1) * 128] = 1.0
    sh["sel"] = sel
    return sh


def prep_core(inp, b, rev=False):
    m = {}
    dsl = slice(None, None, -1) if rev else slice(None)
    m["xT"] = np.ascontiguousarray(np.concatenate([inp["ctx"][b][dsl].T, inp["x"][b][dsl].T], axis=1), dtype=np.float32)
    cp = np.stack([_fm(inp["c"][b]), _fm(inp["c_ctx"])], axis=-1)
    m["c_pk"] = np.ascontiguousarray(cp.reshape(128, 16), dtype=np.float32)
    return m


_NC_CACHE = {}


def kernel(**inputs):
    inp = {k: np.asarray(v) for k, v in inputs.items()}
    if "full" not in _NC_CACHE:
        _NC_CACHE["full"] = build()
    nc = _NC_CACHE["full"]
    shs = [prep_shared(inp, False), prep_shared(inp, True)]
    in_maps = []
    for core in range(8):
        rev = core >= 4
        m = dict(shs[int(rev)]); m.update(prep_core(inp, core % 4, rev)); in_maps.append(m)
    res = run_bass_kernel_spmd(nc, in_maps, core_ids=list(range(8)))
    out = np.empty((4, SEQ, D), np.float32)
    for b in range(4):
        out[b, :HALF] = res.results[b]["outT"].T
        out[b, HALF:] = res.results[b + 4]["outT"].T[::-1]
    return out
```
